# Optimizing a Trainium2 kernel written in Bass

```python
import math
import jax, jax.numpy as jnp
from jax import lax
import numpy as np

D_MODEL = 2048
BATCH = 2
SEQ = 8192
DEPTH = 2
DEC_BATCH = 2
DEC_SEQ = 16384
PAST_LEN = 128

GRID_W = 64
N_MEM = 256
EPS = 1e-6
ATT_HEADS = 8
ATT_KV_HEADS = 2
GQA_GROUP = ATT_HEADS // ATT_KV_HEADS
HEAD_DIM = 128
ATT_W = ATT_HEADS * HEAD_DIM
ATT_KV_W = ATT_KV_HEADS * HEAD_DIM
Q_BLOCK = 128
ROPE_THETA = 10000.0
HG_HEADS = 8
HG_DK = 128
HG_DV = 128
HG_KW = HG_HEADS * HG_DK
HG_VW = HG_HEADS * HG_DV
HG_CHUNK = 64
S5_GROUP = 16
S5_GROUPS = 48
S5_W = S5_GROUPS * S5_GROUP
S5_P = 64
S5_DT_MIN = 0.001
S5_DT_MAX = 0.1
N_BRANCH = 3
XA_HEADS = 4
XA_HEAD_DIM = D_MODEL // XA_HEADS
D_FF = 5632
N_NORMS = 9
IN_SIZES = (ATT_W, ATT_KV_W, ATT_KV_W, HG_KW, HG_KW, HG_KW, HG_VW, HG_VW, S5_W, N_BRANCH * D_MODEL)
IN_SPLITS = tuple(int(v) for v in np.cumsum(IN_SIZES)[:-1])
IN_W = int(sum(IN_SIZES))

kernel_name = 'hybrid_bidir_encoder_gqa_hgrn2_s5'


def rms_norm(x, gain):
    xf = x.astype(jnp.float32)
    y = xf * lax.rsqrt(jnp.mean(xf * xf, axis=-1, keepdims=True) + EPS)
    return (y * gain.astype(jnp.float32)).astype(x.dtype)


def swiglu(x, w_gu, w_down):
    gate, up = jnp.split(x @ w_gu, 2, axis=-1)
    return (jax.nn.silu(gate) * up) @ w_down


def axial_rope_tables(n_tok):
    rows = n_tok // GRID_W
    row = jnp.repeat(jnp.arange(rows, dtype=jnp.float32), GRID_W)
    col = jnp.tile(jnp.arange(GRID_W, dtype=jnp.float32), rows)
    axis_dim = HEAD_DIM // 2
    inv_freq = ROPE_THETA ** (-jnp.arange(0, axis_dim, 2, dtype=jnp.float32) / axis_dim)
    ang = jnp.concatenate([row[:, None] * inv_freq, col[:, None] * inv_freq], axis=-1)
    return jnp.cos(ang), jnp.sin(ang)


def apply_axial_rope(x, cos, sin):
    xf = x.astype(jnp.float32)

    def rot(xp, c, s):
        x1, x2 = jnp.split(xp, 2, axis=-1)
        return jnp.concatenate([x1 * c - x2 * s, x2 * c + x1 * s], axis=-1)

    x_row, x_col = jnp.split(xf, 2, axis=-1)
    c_row, c_col = jnp.split(cos, 2, axis=-1)
    s_row, s_col = jnp.split(sin, 2, axis=-1)
    return jnp.concatenate([rot(x_row, c_row, s_row), rot(x_col, c_col, s_col)], axis=-1).astype(x.dtype)


def axial_gqa(q, k, v, q_gain, k_gain, cos, sin):
    b, n, _ = q.shape
    q = rms_norm(q.reshape(b, n, ATT_KV_HEADS, GQA_GROUP, HEAD_DIM), q_gain)
    k = rms_norm(k.reshape(b, n, ATT_KV_HEADS, HEAD_DIM), k_gain)
    v = v.reshape(b, n, ATT_KV_HEADS, HEAD_DIM)
    q = apply_axial_rope(q, cos[:, None, None, :], sin[:, None, None, :])
    k = apply_axial_rope(k, cos[:, None, :], sin[:, None, :])
    nblk = n // Q_BLOCK
    qb = q.reshape(b, nblk, Q_BLOCK, ATT_KV_HEADS, GQA_GROUP, HEAD_DIM).transpose(1, 0, 3, 4, 2, 5)
    scale = HEAD_DIM ** -0.5

    def block(qi):
        s = jnp.einsum('bhgqd,bkhd->bhgqk', qi, k).astype(jnp.float32) * scale
        p = jax.nn.softmax(s, axis=-1).astype(v.dtype)
        return jnp.einsum('bhgqk,bkhd->bhgqd', p, v)

    o = lax.map(block, qb)
    return o.transpose(1, 0, 4, 2, 3, 5).reshape(b, n, ATT_W)


def hgrn_lower_bounds(logits):
    p = jax.nn.softmax(logits.astype(jnp.float32), axis=0)
    return jnp.cumsum(p, axis=0) - p[0]


def hgrn2_bidir(q, f_fw, f_bw, v, g, lb, o_gain):
    f32 = jnp.float32
    b, n, _ = q.shape
    nc = n // HG_CHUNK
    qd = jnp.stack([q, jnp.flip(q, 1)]).astype(f32)
    vd = jnp.stack([v, jnp.flip(v, 1)]).astype(f32)
    fz = jnp.stack([f_fw, jnp.flip(f_bw, 1)]).astype(f32)
    lbb = lb.astype(f32)[:, None, None, :]
    f = lbb + (1.0 - lbb) * jax.nn.sigmoid(fz)
    logf = jnp.log(f)
    kk = 1.0 - f

    def chunks(a, d):
        return a.reshape(2, b, nc, HG_CHUNK, HG_HEADS, d).transpose(2, 0, 1, 4, 3, 5)

    tri = jnp.tril(jnp.ones((HG_CHUNK, HG_CHUNK), dtype=bool))[:, :, None]

    def step(state, inp):
        qc, kc, vc, lc = inp
        cum = jnp.cumsum(lc, axis=-2)
        diff = cum[..., :, None, :] - cum[..., None, :, :]
        decay = jnp.exp(jnp.where(tri, diff, -jnp.inf))
        scores = jnp.einsum('zbhtk,zbhtsk,zbhsk->zbhts', qc, decay, kc)
        o = (jnp.einsum('zbhts,zbhsv->zbhtv', scores, vc)
             + jnp.einsum('zbhtk,zbhkv->zbhtv', qc * jnp.exp(cum), state))
        last = cum[..., -1:, :]
        state = (state * jnp.exp(last[..., 0, :])[..., None]
                 + jnp.einsum('zbhsk,zbhsv->zbhkv', kc * jnp.exp(last - cum), vc))
        return state, o

    s0 = jnp.zeros((2, b, HG_HEADS, HG_DK, HG_DV), f32)
    _, o = lax.scan(step, s0, (chunks(qd, HG_DK), chunks(kk, HG_DK), chunks(vd, HG_DV), chunks(logf, HG_DK)))
    o = o.transpose(1, 2, 0, 4, 3, 5).reshape(2, b, n, HG_HEADS, HG_DV)
    o = o[0] + jnp.flip(o[1], 1)
    o = rms_norm(o, o_gain.reshape(HG_HEADS, HG_DV)).reshape(b, n, HG_VW)
    return (o * jax.nn.silu(g.astype(f32))).astype(q.dtype)


def _complex_affine_combine(e1, e2):
    a1r, a1i, b1r, b1i = e1
    a2r, a2i, b2r, b2i = e2
    return (a2r * a1r - a2i * a1i,
            a2r * a1i + a2i * a1r,
            a2r * b1r - a2i * b1i + b2r,
            a2r * b1i + a2i * b1r + b2i)


def s5_bidir(u, lam_re, lam_im, log_dt, b_re, b_im, c_re, c_im, d_skip, w_glu):
    f32 = jnp.float32
    b, n, _ = u.shape
    uf = u.astype(f32)
    ug = uf.reshape(b, n, S5_GROUPS, S5_GROUP)
    y = (d_skip.astype(f32) * uf).reshape(b, n, S5_GROUPS, S5_GROUP)
    br = b_re.astype(f32)
    bi = b_im.astype(f32)
    for z in range(2):
        lr = lam_re[z].astype(f32)
        li = lam_im[z].astype(f32)
        dt = jnp.exp(log_dt[z].astype(f32))[:, None]
        mag = jnp.exp(lr * dt)
        ar = mag * jnp.cos(li * dt)
        ai = mag * jnp.sin(li * dt)
        den = lr * lr + li * li
        zr = ((ar - 1.0) * lr + ai * li) / den
        zi = (ai * lr - (ar - 1.0) * li) / den
        bbr = zr[..., None] * br - zi[..., None] * bi
        bbi = zr[..., None] * bi + zi[..., None] * br
        bur = jnp.einsum('gpc,blgc->blgp', bbr, ug)
        bui = jnp.einsum('gpc,blgc->blgp', bbi, ug)
        a_r = jnp.broadcast_to(ar, (1, n, S5_GROUPS, S5_P))
        a_i = jnp.broadcast_to(ai, (1, n, S5_GROUPS, S5_P))
        _, _, hr, hi = lax.associative_scan(_complex_affine_combine, (a_r, a_i, bur, bui),
                                            reverse=(z == 1), axis=1)
        y = (y + jnp.einsum('gcp,blgp->blgc', c_re[z].astype(f32), hr)
             - jnp.einsum('gcp,blgp->blgc', c_im[z].astype(f32), hi))
    y = jax.nn.gelu(y.reshape(b, n, S5_W)).astype(u.dtype)
    val, gate = jnp.split(y @ w_glu, 2, axis=-1)
    return val * jax.nn.sigmoid(gate)


def memory_cross_attention(hn, memn, w_q, w_kv, w_o):
    b, n, _ = hn.shape
    q = (hn @ w_q).reshape(b, n, XA_HEADS, XA_HEAD_DIM)
    k, v = jnp.split(memn @ w_kv, 2, axis=-1)
    k = k.reshape(b, -1, XA_HEADS, XA_HEAD_DIM)
    v = v.reshape(b, -1, XA_HEADS, XA_HEAD_DIM)
    s = jnp.einsum('bnhd,bmhd->bhnm', q, k).astype(jnp.float32) * (XA_HEAD_DIM ** -0.5)
    p = jax.nn.softmax(s, axis=-1).astype(v.dtype)
    o = jnp.einsum('bhnm,bmhd->bnhd', p, v).reshape(b, n, D_MODEL)
    return o @ w_o


def encoder_layer(x, mem, cos, sin, lb, w):
    g = w['norm_gains']
    b, n, _ = x.shape
    h = x + 0.5 * rms_norm(swiglu(rms_norm(x, g[0]), w['ffn_w_gu'][0], w['ffn_w_down'][0]), g[1])
    u = rms_norm(h, g[2])
    (aq, ak, av, hq, hf_fw, hf_bw, hi, hg, su, gate_pre) = jnp.split(u @ w['w_in'], IN_SPLITS, axis=-1)
    y_att = axial_gqa(aq, ak, av, w['att_qk_gain'][0], w['att_qk_gain'][1], cos, sin)
    y_hg = hgrn2_bidir(hq, hf_fw, hf_bw, hi, hg, lb, w['hg_o_gain'])
    y_s5 = s5_bidir(su, w['s5_lam_re'], w['s5_lam_im'], w['s5_log_dt'], w['s5_b_re'], w['s5_b_im'],
                    w['s5_c_re'], w['s5_c_im'], w['s5_d'], w['s5_w_glu'])
    gates = jax.nn.sigmoid(gate_pre.astype(jnp.float32)).astype(h.dtype).reshape(b, n, N_BRANCH, D_MODEL)
    merged = (gates[:, :, 0, :] * (y_att @ w['w_br_att'])
              + gates[:, :, 1, :] * (y_hg @ w['w_br_hg'])
              + gates[:, :, 2, :] * (y_s5 @ w['w_br_s5']))
    h = h + rms_norm(merged @ w['w_out'], g[3])
    h = h + rms_norm(memory_cross_attention(rms_norm(h, g[4]), rms_norm(mem, g[6]),
                                            w['xa_w_q'], w['xa_w_kv'], w['xa_w_o']), g[5])
    h = h + 0.5 * rms_norm(swiglu(rms_norm(h, g[7]), w['ffn_w_gu'][1], w['ffn_w_down'][1]), g[8])
    return h


def run_trunk(x, mem, weights, lb_all):
    cos, sin = axial_rope_tables(x.shape[1])
    for l in range(DEPTH):
        lw = {name: arr[l] for name, arr in weights.items()}
        x = encoder_layer(x, mem, cos, sin, lb_all[l], lw)
    return x


def setup_inputs(seed: int = 0) -> dict:
    key = jax.random.key(seed)
    keys = iter(jax.random.split(key, 40))
    f32 = jnp.float32

    def nrm(shape, fan_in):
        return jax.random.normal(next(keys), shape, f32) * (fan_in ** -0.5)

    def gain(shape):
        return 1.0 + 0.02 * jax.random.normal(next(keys), shape, f32)

    lam_im_base = jnp.pi * jnp.arange(S5_P, dtype=f32)
    return {
        'x_prompt': jax.random.normal(next(keys), (BATCH, SEQ, D_MODEL), f32),
        'x_sample': jax.random.normal(next(keys), (DEC_BATCH, DEC_SEQ, D_MODEL), f32),
        'mem_prompt': jax.random.normal(next(keys), (BATCH, N_MEM, D_MODEL), f32),
        'mem_sample': jax.random.normal(next(keys), (DEC_BATCH, N_MEM, D_MODEL), f32),
        'norm_gains': gain((DEPTH, N_NORMS, D_MODEL)),
        'ffn_w_gu': nrm((DEPTH, 2, D_MODEL, 2 * D_FF), D_MODEL),
        'ffn_w_down': nrm((DEPTH, 2, D_FF, D_MODEL), D_FF),
        'w_in': nrm((DEPTH, D_MODEL, IN_W), D_MODEL),
        'att_qk_gain': gain((DEPTH, 2, HEAD_DIM)),
        'hg_lb_logits': jax.random.normal(next(keys), (DEPTH, 2, HG_KW), f32),
        'hg_o_gain': gain((DEPTH, HG_VW)),
        's5_lam_re': -0.5 + 0.01 * jax.random.normal(next(keys), (DEPTH, 2, S5_GROUPS, S5_P), f32),
        's5_lam_im': lam_im_base + 0.01 * jax.random.normal(next(keys), (DEPTH, 2, S5_GROUPS, S5_P), f32),
        's5_log_dt': jax.random.uniform(next(keys), (DEPTH, 2, S5_GROUPS), f32,
                                        minval=math.log(S5_DT_MIN), maxval=math.log(S5_DT_MAX)),
        's5_b_re': nrm((DEPTH, S5_GROUPS, S5_P, S5_GROUP), 2 * S5_GROUP),
        's5_b_im': nrm((DEPTH, S5_GROUPS, S5_P, S5_GROUP), 2 * S5_GROUP),
        's5_c_re': nrm((DEPTH, 2, S5_GROUPS, S5_GROUP, S5_P), 2 * S5_P),
        's5_c_im': nrm((DEPTH, 2, S5_GROUPS, S5_GROUP, S5_P), 2 * S5_P),
        's5_d': jax.random.normal(next(keys), (DEPTH, S5_W), f32),
        's5_w_glu': nrm((DEPTH, S5_W, 2 * S5_W), S5_W),
        'w_br_att': nrm((DEPTH, ATT_W, D_MODEL), ATT_W),
        'w_br_hg': nrm((DEPTH, HG_VW, D_MODEL), HG_VW),
        'w_br_s5': nrm((DEPTH, S5_W, D_MODEL), S5_W),
        'w_out': nrm((DEPTH, D_MODEL, D_MODEL), D_MODEL),
        'xa_w_q': nrm((DEPTH, D_MODEL, D_MODEL), D_MODEL),
        'xa_w_kv': nrm((DEPTH, D_MODEL, 2 * D_MODEL), D_MODEL),
        'xa_w_o': nrm((DEPTH, D_MODEL, D_MODEL), D_MODEL),
    }


def reference(x_prompt, x_sample, mem_prompt, mem_sample, norm_gains, ffn_w_gu, ffn_w_down, w_in,
              att_qk_gain, hg_lb_logits, hg_o_gain, s5_lam_re, s5_lam_im, s5_log_dt, s5_b_re, s5_b_im,
              s5_c_re, s5_c_im, s5_d, s5_w_glu, w_br_att, w_br_hg, w_br_s5, w_out, xa_w_q, xa_w_kv, xa_w_o):
    weights = {
        'norm_gains': norm_gains, 'ffn_w_gu': ffn_w_gu, 'ffn_w_down': ffn_w_down, 'w_in': w_in,
        'att_qk_gain': att_qk_gain, 'hg_o_gain': hg_o_gain,
        's5_lam_re': s5_lam_re, 's5_lam_im': s5_lam_im, 's5_log_dt': s5_log_dt,
        's5_b_re': s5_b_re, 's5_b_im': s5_b_im, 's5_c_re': s5_c_re, 's5_c_im': s5_c_im,
        's5_d': s5_d, 's5_w_glu': s5_w_glu,
        'w_br_att': w_br_att, 'w_br_hg': w_br_hg, 'w_br_s5': w_br_s5, 'w_out': w_out,
        'xa_w_q': xa_w_q, 'xa_w_kv': xa_w_kv, 'xa_w_o': xa_w_o,
    }
    lb_all = hgrn_lower_bounds(hg_lb_logits)
    y_prompt = run_trunk(x_prompt, mem_prompt, weights, lb_all)
    y_sample = run_trunk(x_sample, mem_sample, weights, lb_all)
    return (y_prompt, y_sample)
```

```python
import math
from contextlib import ExitStack

import numpy as np
import ml_dtypes

import concourse.bass as bass
import concourse.mybir as mybir
from concourse.bass_utils import run_bass_kernel_spmd

F32 = mybir.dt.float32
BF16 = mybir.dt.bfloat16
AF = mybir.ActivationFunctionType
ALU = mybir.AluOpType

D = 2048
DC = 16
DFF = 5632
FC = 44
T = 512
NMEM = 256
INW = 13568
EPS = 1e-6
DEPTH = 2
L5 = 128
HC = 32
ENG = ("pe", "act", "dve", "pool", "sp")

OFF_AQ, OFF_AK, OFF_AV, OFF_HQ, OFF_FF, OFF_FB, OFF_HI, OFF_HG, OFF_SU, OFF_GT = 0, 8, 10, 12, 20, 28, 36, 44, 52, 58


class Prog:
    def __init__(self, nc):
        self.nc = nc
        self.stack = ExitStack()
        self.sems = {}
        self.cnt = {}
        self.nblk = 0
        self.reset_phase()

    def sem(self, key):
        if key not in self.sems:
            self.sems[key] = self.stack.enter_context(self.nc.semaphore("s_" + key))
            self.cnt[key] = 0
        return self.sems[key]

    def reset_phase(self):
        self.streams = {e: [] for e in ENG}
        self.waited = {e: {} for e in ENG}
        self.lastw = {}
        self.readers = {}

    def op(self, eng, fn, reads=(), writes=(), dma=None):
        need = {}

        def req(k, v):
            if need.get(k, 0) < v:
                need[k] = v

        for b in reads:
            lw = self.lastw.get(b)
            if lw is not None:
                req(*lw)
        for b in writes:
            lw = self.lastw.get(b)
            if lw is not None:
                req(*lw)
            for k, v in self.readers.get(b, {}).items():
                req(k, v)
        st = self.streams[eng]
        wd = self.waited[eng]
        for k, v in need.items():
            if k == "pe" and eng == "pe":
                continue
            if wd.get(k, 0) < v:
                st.append(("w", k, v))
                wd[k] = v
        semk = dma if dma is not None else eng
        self.sem(semk)
        inc = 16 if dma is not None else 1
        self.cnt[semk] += inc
        val = self.cnt[semk]
        st.append(("o", fn, semk, inc))
        for b in reads:
            self.readers.setdefault(b, {})[semk] = val
        for b in writes:
            self.lastw[b] = (semk, val)
            self.readers[b] = {}

    def dma(self, q, out, in_, reads, writes, group, slow=False):
        if slow:
            fn = lambda e: e.dma_start(out=out, in_=in_, allow_slow_non_contiguous=True)
        else:
            fn = lambda e: e.dma_start(out=out, in_=in_)
        self.op(q, fn, reads, writes, dma=group)

    def emit(self):
        st = self.streams["sp"]
        wd = self.waited["sp"]
        for k, v in self.cnt.items():
            if v > 0 and wd.get(k, 0) < v:
                st.append(("w", k, v))
                wd[k] = v
        sems = self.sems
        self.nblk += 1
        with self.nc.Block() as block:
            for e, dec in (("pe", block.tensor), ("act", block.scalar), ("dve", block.vector),
                           ("pool", block.gpsimd), ("sp", block.sync)):
                items = self.streams[e]
                if not items:
                    continue

                def body(engine, items=items):
                    for it in items:
                        if it[0] == "w":
                            engine.wait_ge(sems[it[1]], it[2])
                        else:
                            it[1](engine).then_inc(sems[it[2]], it[3])

                dec(body)
        self.reset_phase()


class Ring:
    def __init__(self, items):
        self.items = items
        self.i = 0

    def next(self):
        it = self.items[self.i % len(self.items)]
        self.i += 1
        return it


class TL:
    def __init__(self, h, key):
        self.h = h
        self.key = key
        self.g = "g_" + key


def build(N, debug=()):
    NT = N // T
    HALF = N // 2
    nc = bass.Bass("TRN2", target_bir_lowering=False)
    P = Prog(nc)
    top = P.stack

    def din(name, shape, dt=F32):
        return nc.dram_tensor(name, list(shape), dt, kind="ExternalInput")

    def dscr(name, shape, dt=F32):
        kind = "ExternalOutput" if name in debug else "Internal"
        return nc.dram_tensor(name, list(shape), dt, kind=kind)

    x_in = din("x", [N, D])
    mem_in = din("mem", [2, NMEM, D])
    cos_in = din("cos_t", [128, N])
    sin_in = din("sin_t", [128, N])
    maskb_in = din("maskb", [128, 4])
    rst_in = din("rst", [128, 1])
    cst_in = din("consts", [128, 704])
    w = {}
    w["norm_gains"] = din("norm_gains", [DEPTH, 9, D])
    w["ffn_w_gu"] = din("ffn_w_gu", [DEPTH, 2, D, 2 * DFF])
    w["ffn_w_down"] = din("ffn_w_down", [DEPTH, 2, DFF, D])
    w["w_in"] = din("w_in", [DEPTH, D, INW])
    w["att_qk_gain"] = din("att_qk_gain", [DEPTH, 2, 128])
    w["hg_lb_logits"] = din("hg_lb_logits", [DEPTH, 2, 1024])
    w["hg_o_gain"] = din("hg_o_gain", [DEPTH, 1024])
    w["s5_lam_re"] = din("s5_lam_re", [DEPTH, 2, 48, 64])
    w["s5_lam_im"] = din("s5_lam_im", [DEPTH, 2, 48, 64])
    w["s5_log_dt"] = din("s5_log_dt", [DEPTH, 2, 48])
    w["s5_b_re"] = din("s5_b_re", [DEPTH, 48, 64, 16])
    w["s5_b_im"] = din("s5_b_im", [DEPTH, 48, 64, 16])
    w["s5_c_re"] = din("s5_c_re", [DEPTH, 2, 48, 16, 64])
    w["s5_c_im"] = din("s5_c_im", [DEPTH, 2, 48, 16, 64])
    w["s5_d"] = din("s5_d", [DEPTH, 768])
    w["s5_w_glu"] = din("s5_w_glu", [DEPTH, 768, 1536])
    w["w_br_att"] = din("w_br_att", [DEPTH, 1024, D])
    w["w_br_hg"] = din("w_br_hg", [DEPTH, 1024, D])
    w["w_br_s5"] = din("w_br_s5", [DEPTH, 768, D])
    w["w_out"] = din("w_out", [DEPTH, D, D])
    w["xa_w_q"] = din("xa_w_q", [DEPTH, D, D])
    w["xa_w_kv"] = din("xa_w_kv", [DEPTH, D, 2 * D])
    w["xa_w_o"] = din("xa_w_o", [DEPTH, D, D])
    y_out = nc.dram_tensor("y", [N, D], F32, kind="ExternalOutput")

    hT = dscr("hT", [DC, 128, N])
    WB = {}

    def wb_alloc(name, l, i, K, Fo):
        WB[(name, l, i)] = dscr("wb_%s_%d_%d" % (name, l, i), [Fo // 128, 128, K // 128, 128], BF16)

    for l in range(DEPTH):
        for i in range(2):
            wb_alloc("ffn_w_gu", l, i, D, 2 * DFF)
            wb_alloc("ffn_w_down", l, i, DFF, D)
        wb_alloc("w_in", l, 0, D, INW)
        wb_alloc("s5_w_glu", l, 0, 768, 1536)
        wb_alloc("w_br_att", l, 0, 1024, D)
        wb_alloc("w_br_hg", l, 0, 1024, D)
        wb_alloc("w_br_s5", l, 0, 768, D)
        wb_alloc("w_out", l, 0, D, D)
        wb_alloc("xa_w_q", l, 0, D, D)
        wb_alloc("xa_w_kv", l, 0, D, 2 * D)
        wb_alloc("xa_w_o", l, 0, D, D)

    qT = dscr("qT", [8, 128, N], BF16)
    kT = dscr("kT", [2, 128, N], BF16)
    vtm = dscr("vtm", [2, N, 128], BF16)
    hqT = dscr("hqT", [8, 128, N])
    lfT = dscr("lfT", [2, 8, 128, N])
    kkT = dscr("kkT", [2, 8, 128, N])
    hvtm = dscr("hvtm", [N, 1024], BF16)
    sgT = dscr("sgT", [8, 128, N])
    suT = dscr("suT", [6, 128, N])
    gtT = [dscr("gtT%d" % b_, [DC, 128, N]) for b_ in range(3)]
    yattT = dscr("yattT", [8, 128, N], BF16)
    ohT = dscr("ohT", [8, 128, N])
    ysT = dscr("ysT", [6, 128, N], BF16)
    ysT_f = dscr("ysT_f", [6, 128, N])

    uid = [0]

    def sb(name, shape, dt=F32, stack=None):
        uid[0] += 1
        h = (stack or top).enter_context(nc.sbuf_tensor("%s_%d" % (name, uid[0]), list(shape), dt))
        return TL(h, name)

    psb = [TL(top.enter_context(nc.psum_tensor("ps%d" % i, [128, 512], F32)), "ps%d" % i) for i in range(7)]
    pst = TL(top.enter_context(nc.psum_tensor("pst", [128, 1024], BF16)), "pst")
    cst = sb("cst", [128, 704])
    cstb = sb("cstb", [128, 704], BF16)
    G = sb("G", [128, DEPTH * 9 * DC])
    epsc = sb("epsc", [128, 1])
    ident = cst.h[:, 0:128]
    identb = cstb.h[:, 0:128]
    onesb = cstb.h[:, 128:256]
    rotTb = cstb.h[:, 256:384]
    QKG = sb("QKG", [128, 4])
    BM = sb("BM", [128, 8])
    LGT = sb("LGT", [128, 32])
    LB = sb("LB", [128, 32])
    OML = sb("OML", [128, 32])
    OG = sb("OG", [128, 16])
    RST = sb("RST", [128, 1])
    MB = sb("MB", [128, 4])
    S5D = sb("S5D", [128, 12])

    def gain(l, n, c):
        o = (l * 9 + n) * DC + c
        return G.h[:, o:o + 1]

    P.dma("sp", cst.h[:, :], cst_in[:, :], [], ["cst"], cst.g)
    P.op("dve", lambda e: e.tensor_copy(out=cstb.h[:, :], in_=cst.h[:, :]), ["cst"], ["cstb"])
    P.op("dve", lambda e: e.memset(epsc.h[:, :], EPS), [], ["epsc"])
    for l in range(DEPTH):
        for n in range(9):
            o = (l * 9 + n) * DC
            P.dma("sp", G.h[:, o:o + DC], w["norm_gains"].ap()[l, n].rearrange("(c p) -> p c", p=128),
                  [], ["G"], G.g, slow=True)

    P.dma("sp", RST.h[:, :], rst_in[:, :], [], ["RST"], RST.g)
    P.dma("sp", MB.h[:, :], maskb_in[:, :], [], ["MB"], MB.g)
    for l in range(DEPTH):
        for i in range(2):
            P.dma("sp", QKG.h[:, l * 2 + i:l * 2 + i + 1], w["att_qk_gain"].ap()[l, i].rearrange("(p o) -> p o", o=1), [], ["QKG"], QKG.g, slow=True)
        for z in range(2):
            o = (l * 2 + z) * 8
            P.dma("sp", LGT.h[:, o:o + 8], w["hg_lb_logits"].ap()[l, z].rearrange("(h p) -> p h", p=128), [], ["LGT"], LGT.g, slow=True)
        P.dma("sp", OG.h[:, l * 8:(l + 1) * 8], w["hg_o_gain"].ap()[l].rearrange("(h p) -> p h", p=128), [], ["OG"], OG.g, slow=True)
        P.dma("sp", S5D.h[:, l * 6:(l + 1) * 6], w["s5_d"].ap()[l].rearrange("(c p) -> p c", p=128), [], ["S5D"], S5D.g, slow=True)
    P.op("dve", lambda e: e.memset(LB.h[:, 0:16], 0.0), [], ["LB"])
    P.op("dve", lambda e: e.tensor_tensor(out=LB.h[:, 16:32], in0=LGT.h[:, 16:32], in1=LGT.h[:, 0:16], op=ALU.subtract), ["LGT", "LB"], ["LB"])
    P.op("act", lambda e: e.activation(out=LB.h[:, 16:32], in_=LB.h[:, 16:32], func=AF.Sigmoid), ["LB"], ["LB"])
    P.op("dve", lambda e: e.tensor_scalar(out=OML.h[:, :], in0=LB.h[:, :], scalar1=-1.0, scalar2=1.0, op0=ALU.mult, op1=ALU.add), ["LB"], ["OML"])
    for l in range(DEPTH):
        pv = sb("pv%d" % l, [128, 1])
        pm = sb("pm%d" % l, [1, 2])
        P.op("dve", lambda e, l=l, pv=pv: e.tensor_tensor(out=pv.h[:, :], in0=QKG.h[:, 2 * l:2 * l + 1], in1=QKG.h[:, 2 * l + 1:2 * l + 2], op=ALU.mult), ["QKG"], [pv.key])
        P.op("pe", lambda e, pv=pv: e.transpose(out=psb[0].h[0:1, 0:128], in_=pv.h[:, 0:1], identity=ident), [pv.key, "cst"], ["ps0"])
        P.op("dve", lambda e, pm=pm: e.tensor_reduce(out=pm.h[0:1, 0:1], in_=psb[0].h[0:1, 0:128], axis=mybir.AxisListType.X, op=ALU.max, apply_absolute_value=True), ["ps0"], [pm.key])
        P.op("pe", lambda e, pm=pm: e.matmul(psb[1].h[:, 0:1], lhsT=cst.h[0:1, 128:256], rhs=pm.h[0:1, 0:1], start=True, stop=True), [pm.key, "cst"], ["ps1"])
        P.op("dve", lambda e, pv=pv: e.tensor_scalar(out=pv.h[:, :], in0=psb[1].h[:, 0:1], scalar1=-math.sqrt(128.0), scalar2=-0.5, op0=ALU.mult, op1=ALU.add), ["ps1"], [pv.key])
        P.op("dve", lambda e, l=l, pv=pv: e.tensor_scalar(out=BM.h[:, 4 * l:4 * l + 4], in0=MB.h[:, :], scalar1=pv.h[:, 0:1], scalar2=None, op0=ALU.add), [pv.key, "MB"], ["BM"])

    with ExitStack() as ph:
        stg = [sb("stg%d" % i, [128, 8, 256], F32, ph) for i in range(3)]
        stb = [sb("stb%d" % i, [128, 2, 8, 128], BF16, ph) for i in range(3)]
        ring_i = [0]
        cast_eng = Ring(["dve", "act", "dve", "pool"])

        def convert(name, l, i, K, Fo):
            src = w[name].ap()[l, i] if len(w[name].shape) == 4 else w[name].ap()[l]
            dst = WB[(name, l, i)]
            KC = K // 128
            for s in range(Fo // 256):
                for kc0 in range(0, KC, 8):
                    kg = min(8, KC - kc0)
                    a = stg[ring_i[0] % 3]
                    b = stb[ring_i[0] % 3]
                    ring_i[0] += 1
                    P.dma("sp", a.h[:, :kg, :],
                          src[kc0 * 128:(kc0 + kg) * 128, s * 256:(s + 1) * 256].rearrange("(kc p) c -> p kc c", p=128),
                          [], [a.key], a.g)
                    ce = cast_eng.next()
                    iv = a.h[:, :kg, :].rearrange("p kc (j c) -> p j kc c", c=128)
                    ov = b.h[:, :, :kg, :]
                    if ce == "act":
                        P.op("act", lambda e, ov=ov, iv=iv: e.activation(out=ov, in_=iv, func=AF.Copy), [a.key], [b.key])
                    else:
                        P.op(ce, lambda e, ov=ov, iv=iv: e.tensor_copy(out=ov, in_=iv), [a.key], [b.key])
                    P.dma("act", dst[2 * s:2 * s + 2, :, kc0:kc0 + kg, :].rearrange("j p kc c -> p j kc c"), ov,
                          [b.key], [], b.g)

        for l in range(DEPTH):
            for i in range(2):
                convert("ffn_w_gu", l, i, D, 2 * DFF)
                convert("ffn_w_down", l, i, DFF, D)
            convert("w_in", l, 0, D, INW)
            convert("s5_w_glu", l, 0, 768, 1536)
            convert("w_br_att", l, 0, 1024, D)
            convert("w_br_hg", l, 0, 1024, D)
            convert("w_br_s5", l, 0, 768, D)
            convert("w_out", l, 0, D, D)
            convert("xa_w_q", l, 0, D, D)
            convert("xa_w_kv", l, 0, D, 2 * D)
            convert("xa_w_o", l, 0, D, D)

        xs = [sb("xs%d" % i, [128, D], F32, ph) for i in range(2)]
        xo = [sb("xo%d" % i, [128, DC, 128], F32, ph) for i in range(2)]
        for tb in range(N // 128):
            a = xs[tb % 2]
            o = xo[tb % 2]
            P.dma("pool", a.h[:, :], x_in[tb * 128:(tb + 1) * 128, :], [], [a.key], a.g)
            for q in range(4):
                ps = psb[(tb * 4 + q) % 7]

                def tr(e, ps=ps, a=a, q=q):
                    for c in range(4):
                        ins = e.transpose(out=ps.h[:, c * 128:(c + 1) * 128], in_=a.h[:, (q * 4 + c) * 128:(q * 4 + c + 1) * 128],
                                          identity=ident)
                    return ins

                P.op("pe", tr, [a.key, "cst"], [ps.key])
                P.op("dve" if q % 2 == 0 else "act",
                     (lambda e, ps=ps, o=o, q=q: e.tensor_copy(out=o.h[:, q * 4:(q + 1) * 4, :], in_=ps.h[:, :].rearrange("p (c t) -> p c t", t=128)))
                     if q % 2 == 0 else
                     (lambda e, ps=ps, o=o, q=q: e.activation(out=o.h[:, q * 4:(q + 1) * 4, :], in_=ps.h[:, :].rearrange("p (c t) -> p c t", t=128), func=AF.Copy)),
                     [ps.key], [o.key])
            P.dma("pool", hT[:, :, tb * 128:(tb + 1) * 128].rearrange("c p t -> p c t"), o.h[:, :, :], [o.key], [], o.g)
        P.emit()

    class Ctx:
        pass

    class V:
        def __init__(self, ap, key, g=None):
            self.h = ap
            self.key = key
            self.g = g or ("g_" + key)

    H1K = ["h1_%d" % j for j in range(FC)]
    YBK = ["yb%d" % c for c in range(DC)]

    def make_linear(cx):
        def linear_chunk(name, l, i, j, KC, xap, xkeys, ncols=T):
            Wd = WB[(name, l, i)]
            pieces = []
            for k0 in range(0, KC, 22):
                kg = min(22, KC - k0)
                s = cx.wr.next()
                P.dma("sp", s.h[:, :kg, :], Wd[j, :, k0:k0 + kg, :], [], [s.key], s.g)
                pieces.append((s, k0, kg))
            ps = cx.psr.next()

            def mm(e):
                ins = None
                for (s, k0, kg) in pieces:
                    for kk in range(kg):
                        kc = k0 + kk
                        ins = e.matmul(ps.h[:, 0:ncols], lhsT=s.h[:, kk, :], rhs=xap(kc), start=(kc == 0), stop=(kc == KC - 1))
                return ins

            P.op("pe", mm, [s.key for (s, _, _) in pieces] + list(xkeys), [ps.key])
            return ps

        return linear_chunk

    def rstd_from(cx, sqap, sqkeys, C, Dn, wgt=1.0, ncols=T):
        ps = cx.psr.next()

        def mm(e):
            for c in range(C):
                ins = e.matmul(ps.h[:, 0:ncols], lhsT=onesb, rhs=sqap(c), start=(c == 0), stop=(c == C - 1))
            return ins

        P.op("pe", mm, list(sqkeys) + ["cstb"], [ps.key])
        rs = cx.rsr.next()
        P.op("act", lambda e: e.activation(out=rs.h[:, 0:ncols], in_=ps.h[:, 0:ncols], func=AF.Sqrt, scale=1.0 / Dn, bias=epsc.h[:, 0:1]),
             [ps.key, "epsc"], [rs.key])
        P.op("dve", lambda e: e.reciprocal(out=rs.h[:, 0:ncols], in_=rs.h[:, 0:ncols]), [rs.key], [rs.key])
        if wgt != 1.0:
            P.op("dve", lambda e: e.tensor_scalar(out=rs.h[:, 0:ncols], in0=rs.h[:, 0:ncols], scalar1=float(wgt), scalar2=None, op0=ALU.mult),
                 [rs.key], [rs.key])
        return rs

    def rms_stats(cx, src, srckeys, C, Dn, wgt=1.0, ncols=T):
        sq = cx.h1
        P.op("act", lambda e: e.activation(out=sq.h[:, :C, 0:ncols], in_=src[:, :C, 0:ncols], func=AF.Square), list(srckeys), H1K[:C])
        return rstd_from(cx, lambda c: sq.h[:, c, 0:ncols], H1K[:C], C, Dn, wgt, ncols)

    def norm_to(cx, src, srckeys, C, gfn, dst, dstkeys, Dn=D, ncols=T):
        rs = rms_stats(cx, src, srckeys, C, Dn, 1.0, ncols)
        for c in range(C):
            P.op("dve", lambda e, c=c: e.scalar_tensor_tensor(out=dst[:, c, 0:ncols], in0=src[:, c, 0:ncols], scalar=gfn(c), in1=rs.h[:, 0:ncols],
                                                               op0=ALU.mult, op1=ALU.mult), list(srckeys) + [rs.key, "G"], list(dstkeys))

    def norm_residual(cx, gfn, hh, wgt):
        yb = cx.yb
        rs = rms_stats(cx, yb.h, YBK, DC, D, wgt)
        for c in range(DC):
            tmp = cx.tmpr.next()
            P.op("dve", lambda e, c=c, tmp=tmp: e.scalar_tensor_tensor(out=tmp.h[:, :], in0=yb.h[:, c, :], scalar=gfn(c), in1=rs.h[:, :],
                                                                        op0=ALU.mult, op1=ALU.mult), [YBK[c], rs.key, "G"], [tmp.key])
            P.op("pool", lambda e, c=c, tmp=tmp: e.tensor_tensor(out=hh.h[:, c, :], in0=tmp.h[:, :], in1=hh.h[:, c, :], op=ALU.add),
                 [tmp.key, hh.key], [hh.key])

    def ffn(cx, l, i, hh, n_pre, n_post):
        lin = cx.lin
        xn, h1, yb = cx.xn, cx.h1, cx.yb
        norm_to(cx, hh.h, [hh.key], DC, lambda c: gain(l, n_pre, c), xn.h, [xn.key])
        xap = lambda kc: xn.h[:, kc, :]
        for j in range(FC):
            pg = lin("ffn_w_gu", l, i, j, DC, xap, [xn.key])
            pu = lin("ffn_w_gu", l, i, FC + j, DC, xap, [xn.key])
            sg = cx.tmpr.next()
            P.op("act", lambda e, sg=sg, pg=pg: e.activation(out=sg.h[:, :], in_=pg.h[:, :], func=AF.Silu), [pg.key], [sg.key])
            P.op("dve", lambda e, sg=sg, pu=pu, j=j: e.tensor_tensor(out=h1.h[:, j, :], in0=sg.h[:, :], in1=pu.h[:, :], op=ALU.mult),
                 [sg.key, pu.key], [H1K[j]])
        hap = lambda kc: h1.h[:, kc, :]
        for c in range(DC):
            pd = lin("ffn_w_down", l, i, c, FC, hap, H1K)
            P.op("act", lambda e, pd=pd, c=c: e.activation(out=yb.h[:, c, :], in_=pd.h[:, :], func=AF.Copy), [pd.key], [YBK[c]])
        norm_residual(cx, lambda c: gain(l, n_post, c), hh, 0.5)

    def tile_ctx(ph):
        cx = Ctx()
        cx.wr = Ring([sb("wsl%d" % i, [128, 22, 128], BF16, ph) for i in range(5)])
        cx.psr = Ring(psb)
        cx.rsr = Ring([sb("rs%d" % i, [128, T], F32, ph) for i in range(2)])
        cx.tmpr = Ring([sb("tmp%d" % i, [128, T], F32, ph) for i in range(3)])
        cx.xn = sb("xn", [128, DC, T], BF16, ph)
        cx.h1 = sb("h1", [128, FC, T], BF16, ph)
        cx.yb = sb("yb", [128, DC, T], F32, ph)
        cx.lin = make_linear(cx)
        cx.hh = sb("hh", [128, DC, T], F32, ph)
        return cx

    def load_h(cx, t):
        P.dma("pool", cx.hh.h[:, :, :], hT[:, :, t * T:(t + 1) * T].rearrange("c p t -> p c t"), [("hT", t)], [cx.hh.key], "g_hh")

    def store_h(cx, t):
        P.dma("pool", hT[:, :, t * T:(t + 1) * T].rearrange("c p t -> p c t"), cx.hh.h[:, :, :], [cx.hh.key], [("hT", t)], "g_hhs")

    def hgrn_phase(l):
        NCH = T // HC
        with ExitStack() as ph:
            smask = sb("smask", [128, T], F32, ph)
            P.op("dve", lambda e: e.memset(smask.h[:, :], 1.0), [], [smask.key])
            P.op("dve", lambda e: e.memset(smask.h[:, :].rearrange("p (c s) -> p c s", s=HC)[:, :, 0:1], 0.0), [smask.key], [smask.key])
            tri = [cst.h[0:HC, 384:384 + HC], cst.h[0:HC, 416:416 + HC]]
            HG = 2
            trb = [pst.h, psb[6].h[:, :].bitcast(BF16)]
            trk = ["pst", "ps6"]
            S = [sb("S%d" % i, [128, 128], F32, ph) for i in range(HG)]
            Sb = [sb("Sb%d" % i, [128, 128], BF16, ph) for i in range(HG)]
            bufs = []
            for i in range(HG):
                b = Ctx()
                b.lf = sb("lf%d" % i, [128, T], F32, ph)
                b.kk = sb("kk%d" % i, [128, T], F32, ph)
                b.q = sb("q%d" % i, [128, T], F32, ph)
                b.pc = sb("pc%d" % i, [128, T], F32, ph)
                b.e1 = sb("e1%d" % i, [128, T], F32, ph)
                b.e2 = sb("e2%d" % i, [128, T], F32, ph)
                b.qd = sb("qd%d" % i, [128, T], BF16, ph)
                b.kd = sb("kd%d" % i, [128, T], BF16, ph)
                b.v = sb("v%d" % i, [HC, NCH, 128], BF16, ph)
                b.ke = Ring([sb("ke%d_%d" % (i, r), [128, HC], BF16, ph) for r in range(2)])
                b.kt = Ring([sb("kt%d_%d" % (i, r), [HC, 128], BF16, ph) for r in range(2)])
                b.sc = Ring([sb("sc%d_%d" % (i, r), [HC, HC], BF16, ph) for r in range(2)])
                b.of = sb("of%d" % i, [128, T], F32, ph)
                b.ob = sb("ob%d" % i, [128, T], F32, ph)
                bufs.append(b)
            for z in range(2):
                for hg in range(8 // HG):
                    for i in range(HG):
                        P.op("dve", lambda e, i=i: e.memset(S[i].h[:, :], 0.0), [], [S[i].key])
                        P.op("pool", lambda e, i=i: e.memset(Sb[i].h[:, :], 0.0), [], [Sb[i].key])
                    torder = range(NT) if z == 0 else range(NT - 1, -1, -1)
                    for t in torder:
                        tsl = slice(t * T, (t + 1) * T)
                        if (z == 0 and t * T == HALF) or (z == 1 and (t + 1) * T == HALF):
                            for i in range(HG):
                                P.op("dve", lambda e, i=i: e.tensor_scalar(out=S[i].h[:, :], in0=S[i].h[:, :], scalar1=RST.h[:, 0:1], scalar2=None, op0=ALU.mult),
                                     [S[i].key, "RST"], [S[i].key])
                                P.op("act", lambda e, i=i: e.activation(out=Sb[i].h[:, :], in_=S[i].h[:, :], func=AF.Copy), [S[i].key], [Sb[i].key])
                        for i in range(HG):
                            b = bufs[i]
                            hd = hg * HG + i
                            P.dma("pool", b.lf.h[:, :], lfT[z, hd, :, tsl], [], [b.lf.key], b.lf.g)
                            P.dma("pool", b.kk.h[:, :], kkT[z, hd, :, tsl], [], [b.kk.key], b.kk.g)
                            P.dma("pool", b.q.h[:, :], hqT[hd, :, tsl], [], [b.q.key], b.q.g)
                            P.dma("pool", b.v.h[:, :, :], hvtm[tsl, hd * 128:(hd + 1) * 128].rearrange("(c s) d -> s c d", s=HC), [], [b.v.key], b.v.g)
                            if z == 1:
                                P.dma("pool", b.of.h[:, :], ohT[hd, :, tsl], [("ohT", hd, t)], [b.of.key], b.of.g)
                            P.op("dve", lambda e, b=b: e.tensor_tensor_scan(out=b.pc.h[:, :], data0=smask.h[:, :], data1=b.lf.h[:, :], initial=0.0,
                                                                           op0=ALU.mult, op1=ALU.add), [smask.key, b.lf.key], [b.pc.key])
                            if z == 1:
                                P.op("pool", lambda e, b=b: e.tensor_tensor(out=b.e2.h[:, :], in0=b.lf.h[:, :], in1=b.pc.h[:, :], op=ALU.subtract),
                                     [b.lf.key, b.pc.key], [b.e2.key])
                                P.op("dve", lambda e, b=b: e.tensor_tensor(
                                    out=b.lf.h[:, :].rearrange("p (c s) -> p c s", s=HC), in0=b.e2.h[:, :].rearrange("p (c s) -> p c s", s=HC),
                                    in1=b.pc.h[:, :].rearrange("p (c s) -> p c s", s=HC)[:, :, HC - 1:HC].broadcast_to([128, NCH, HC]), op=ALU.add),
                                    [b.e2.key, b.pc.key], [b.lf.key])
                            cum = b.pc if z == 0 else b.lf
                            P.op("act", lambda e, b=b, cum=cum: e.activation(out=b.e1.h[:, :], in_=cum.h[:, :], func=AF.Exp), [cum.key], [b.e1.key])
                            P.op("act", lambda e, b=b, cum=cum: e.activation(out=b.e2.h[:, :], in_=cum.h[:, :], func=AF.Exp, scale=-1.0), [cum.key], [b.e2.key])
                            P.op("dve", lambda e, b=b: e.tensor_tensor(out=b.qd.h[:, :], in0=b.q.h[:, :], in1=b.e1.h[:, :], op=ALU.mult), [b.q.key, b.e1.key], [b.qd.key])
                            P.op("pool", lambda e, b=b: e.tensor_tensor(out=b.kd.h[:, :], in0=b.kk.h[:, :], in1=b.e2.h[:, :], op=ALU.mult), [b.kk.key, b.e2.key], [b.kd.key])
                        corder = range(NCH) if z == 0 else range(NCH - 1, -1, -1)
                        for c in corder:
                            csl = slice(c * HC, (c + 1) * HC)
                            epos = c * HC + (HC - 1 if z == 0 else 0)
                            for i in range(HG):
                                b = bufs[i]
                                ecol = b.e1.h[:, epos:epos + 1]
                                ke, kt, sc = b.ke.next(), b.kt.next(), b.sc.next()
                                P.op("pool", lambda e, b=b, ke=ke, ecol=ecol, csl=csl: e.tensor_scalar(out=ke.h[:, :], in0=b.kd.h[:, csl], scalar1=ecol, scalar2=None, op0=ALU.mult),
                                     [b.kd.key, b.e1.key], [ke.key])
                                tk = trk[i]
                                P.op("pe", lambda e, ke=ke, i=i: e.transpose(out=trb[i][0:HC, 0:128], in_=ke.h[:, :], identity=identb),
                                     [ke.key, "cstb"], [tk])
                                P.op("act", lambda e, kt=kt, i=i: e.activation(out=kt.h[:, :], in_=trb[i][0:HC, 0:128], func=AF.Copy), [tk], [kt.key])
                                sk = psb[2 + i].key
                                P.op("pe", lambda e, b=b, i=i, csl=csl: e.matmul(psb[2 + i].h[0:HC, 0:HC], lhsT=b.kd.h[:, csl], rhs=b.qd.h[:, csl], start=True, stop=True),
                                     [b.kd.key, b.qd.key], [sk])
                                P.op("dve", lambda e, sc=sc, i=i, trz=tri[z]: e.tensor_tensor(out=sc.h[:, :], in0=psb[2 + i].h[0:HC, 0:HC], in1=trz, op=ALU.mult),
                                     [sk, "cst"], [sc.key])
                                ok = psb[i].key

                                def om(e, b=b, i=i, sc=sc, c=c, csl=csl):
                                    e.matmul(psb[i].h[:, csl], lhsT=b.v.h[:, c, :], rhs=sc.h[:, :], start=True, stop=False)
                                    return e.matmul(psb[i].h[:, csl], lhsT=Sb[i].h[:, :], rhs=b.qd.h[:, csl], start=False, stop=True)

                                P.op("pe", om, [b.v.key, sc.key, Sb[i].key, b.qd.key], [ok])
                                dk = psb[4 + i].key
                                P.op("pe", lambda e, b=b, i=i, kt=kt, c=c: e.matmul(psb[4 + i].h[:, 0:128], lhsT=kt.h[:, :], rhs=b.v.h[:, c, :], start=True, stop=True),
                                     [kt.key, b.v.key], [dk])
                                P.op("dve", lambda e, i=i, ecol=ecol: e.scalar_tensor_tensor(out=S[i].h[:, :], in0=S[i].h[:, :], scalar=ecol,
                                                                                              in1=psb[4 + i].h[:, 0:128], op0=ALU.mult, op1=ALU.add),
                                     [S[i].key, dk, b.e1.key], [S[i].key])
                                P.op("act", lambda e, i=i: e.activation(out=Sb[i].h[:, :], in_=S[i].h[:, :], func=AF.Copy), [S[i].key], [Sb[i].key])
                        for i in range(HG):
                            b = bufs[i]
                            hd = hg * HG + i
                            ok = psb[i].key
                            if z == 0:
                                P.op("act", lambda e, b=b, i=i: e.activation(out=b.ob.h[:, :], in_=psb[i].h[:, :], func=AF.Copy), [ok], [b.ob.key])
                            else:
                                P.op("dve", lambda e, b=b, i=i: e.tensor_tensor(out=b.ob.h[:, :], in0=psb[i].h[:, :], in1=b.of.h[:, :], op=ALU.add), [ok, b.of.key], [b.ob.key])
                            P.dma("pool", ohT[hd, :, tsl], b.ob.h[:, :], [b.ob.key], [("ohT", hd, t)], b.ob.g)
            P.emit()

    def s5_phase(l):
        NG = 48
        lay5 = ExitStack()
        Bm = sb("Bm", [128, 96, 128], BF16, lay5)
        Cm = sb("Cm", [128, 96, 128], BF16, lay5)
        Ct = sb("Ct", [128, NG, L5], F32, lay5)
        St = sb("St", [128, NG, L5], F32, lay5)
        RHO = sb("RHO", [128, NG], F32, lay5)
        CP = sb("CP", [128, NG], F32, lay5)
        SP = sb("SP", [128, NG], F32, lay5)
        with ExitStack() as ph:
            def t48(name):
                return sb(name, [128, NG], F32, ph)
            LR, LI, DT, TH, X, X2, U, CS, SN, A, B_, ZR, ZI, DEN = [t48(n) for n in
                ("LR", "LI", "DT", "TH", "X", "X2", "U", "CS", "SN", "A", "B_", "ZR", "ZI", "DEN")]
            KI = sb("KI", [128, NG], mybir.dt.int32, ph)
            for z in range(2):
                P.dma("sp", LR.h[:, z * 24:(z + 1) * 24], w["s5_lam_re"].ap()[l, z].rearrange("(gp g2) s -> (g2 s) gp", g2=2), [], [LR.key], LR.g, slow=True)
                P.dma("sp", LI.h[:, z * 24:(z + 1) * 24], w["s5_lam_im"].ap()[l, z].rearrange("(gp g2) s -> (g2 s) gp", g2=2), [], [LI.key], LI.g, slow=True)
                for g2 in range(2):
                    src = bass.AP(w["s5_log_dt"], (l * 2 + z) * 48 + g2, [[0, 64], [2, 24]])
                    P.dma("sp", DT.h[g2 * 64:(g2 + 1) * 64, z * 24:(z + 1) * 24], src, [], [DT.key], DT.g, slow=True)

            def tt(eng, out, a, b, op, rk, wk):
                P.op(eng, lambda e: e.tensor_tensor(out=out, in0=a, in1=b, op=op), rk, wk)

            def ts(eng, out, a, s1, s2, op0, op1, rk, wk):
                if s2 is None:
                    P.op(eng, lambda e: e.tensor_scalar(out=out, in0=a, scalar1=s1, scalar2=None, op0=op0), rk, wk)
                else:
                    P.op(eng, lambda e: e.tensor_scalar(out=out, in0=a, scalar1=s1, scalar2=s2, op0=op0, op1=op1), rk, wk)

            def f(tl):
                return tl.h[:, :]

            P.op("act", lambda e: e.activation(out=f(DT), in_=f(DT), func=AF.Exp), [DT.key], [DT.key])
            tt("dve", f(TH), f(LI), f(DT), ALU.mult, [LI.key, DT.key], [TH.key])
            tt("dve", f(A), f(LR), f(DT), ALU.mult, [LR.key, DT.key], [A.key])
            P.op("act", lambda e: e.activation(out=f(RHO), in_=f(A), func=AF.Exp), [A.key], [RHO.key])
            ts("dve", f(X), f(TH), 1.0 / (2.0 * math.pi), None, ALU.mult, None, [TH.key], [X.key])
            P.op("dve", lambda e: e.tensor_copy(out=KI.h[:, :], in_=f(X)), [X.key], [KI.key])
            P.op("dve", lambda e: e.tensor_copy(out=f(X), in_=KI.h[:, :]), [KI.key], [X.key])
            C1, C2 = 6.28125, 2.0 * math.pi - 6.28125
            P.op("dve", lambda e: e.scalar_tensor_tensor(out=f(U), in0=f(X), scalar=-C1, in1=f(TH), op0=ALU.mult, op1=ALU.add), [X.key, TH.key], [U.key])
            P.op("dve", lambda e: e.scalar_tensor_tensor(out=f(U), in0=f(X), scalar=-C2, in1=f(U), op0=ALU.mult, op1=ALU.add), [X.key, U.key], [U.key])
            ts("dve", f(X), f(U), 0.125, None, ALU.mult, None, [U.key], [X.key])
            tt("dve", f(X2), f(X), f(X), ALU.mult, [X.key], [X2.key])
            ts("dve", f(SN), f(X2), -1.0 / 110.0, 1.0, ALU.mult, ALU.add, [X2.key], [SN.key])
            for cden in (72.0, 42.0, 20.0, 6.0):
                tt("dve", f(SN), f(SN), f(X2), ALU.mult, [SN.key, X2.key], [SN.key])
                ts("dve", f(SN), f(SN), -1.0 / cden, 1.0, ALU.mult, ALU.add, [SN.key], [SN.key])
            tt("dve", f(SN), f(SN), f(X), ALU.mult, [SN.key, X.key], [SN.key])
            ts("dve", f(CS), f(X2), -1.0 / 90.0, 1.0, ALU.mult, ALU.add, [X2.key], [CS.key])
            for cden in (56.0, 30.0, 12.0, 2.0):
                tt("dve", f(CS), f(CS), f(X2), ALU.mult, [CS.key, X2.key], [CS.key])
                ts("dve", f(CS), f(CS), -1.0 / cden, 1.0, ALU.mult, ALU.add, [CS.key], [CS.key])

            def dbl(c, s_):
                tt("dve", f(A), f(c), f(s_), ALU.mult, [c.key, s_.key], [A.key])
                tt("dve", f(B_), f(s_), f(s_), ALU.mult, [s_.key], [B_.key])
                tt("dve", f(c), f(c), f(c), ALU.mult, [c.key], [c.key])
                tt("dve", f(c), f(c), f(B_), ALU.subtract, [c.key, B_.key], [c.key])
                ts("dve", f(s_), f(A), 2.0, None, ALU.mult, None, [A.key], [s_.key])

            for _ in range(3):
                dbl(CS, SN)
            tt("dve", f(A), f(RHO), f(CS), ALU.mult, [RHO.key, CS.key], [A.key])
            ts("dve", f(A), f(A), -1.0, None, ALU.add, None, [A.key], [A.key])
            tt("dve", f(B_), f(RHO), f(SN), ALU.mult, [RHO.key, SN.key], [B_.key])
            tt("dve", f(DEN), f(LR), f(LR), ALU.mult, [LR.key], [DEN.key])
            tt("dve", f(U), f(LI), f(LI), ALU.mult, [LI.key], [U.key])
            tt("dve", f(DEN), f(DEN), f(U), ALU.add, [DEN.key, U.key], [DEN.key])
            P.op("dve", lambda e: e.reciprocal(out=f(DEN), in_=f(DEN)), [DEN.key], [DEN.key])
            tt("dve", f(ZR), f(A), f(LR), ALU.mult, [A.key, LR.key], [ZR.key])
            tt("dve", f(U), f(B_), f(LI), ALU.mult, [B_.key, LI.key], [U.key])
            tt("dve", f(ZR), f(ZR), f(U), ALU.add, [ZR.key, U.key], [ZR.key])
            tt("dve", f(ZR), f(ZR), f(DEN), ALU.mult, [ZR.key, DEN.key], [ZR.key])
            tt("dve", f(ZI), f(B_), f(LR), ALU.mult, [B_.key, LR.key], [ZI.key])
            tt("dve", f(U), f(A), f(LI), ALU.mult, [A.key, LI.key], [U.key])
            tt("dve", f(ZI), f(ZI), f(U), ALU.subtract, [ZI.key, U.key], [ZI.key])
            tt("dve", f(ZI), f(ZI), f(DEN), ALU.mult, [ZI.key, DEN.key], [ZI.key])
            CM, SM = t48("CM"), t48("SM")
            P.op("dve", lambda e: e.tensor_copy(out=f(CM), in_=f(CS)), [CS.key], [CM.key])
            P.op("dve", lambda e: e.tensor_copy(out=f(SM), in_=f(SN)), [SN.key], [SM.key])
            P.op("dve", lambda e: e.memset(Ct.h[:, :, 0:1], 1.0), [], [Ct.key])
            P.op("dve", lambda e: e.memset(St.h[:, :, 0:1], 0.0), [], [St.key])
            TA = sb("TA", [128, NG, L5 // 2], F32, ph)
            TB = sb("TB", [128, NG, L5 // 2], F32, ph)
            m = 1
            while m < L5:
                cb = CM.h[:, :].unsqueeze(2).broadcast_to([128, NG, m])
                sbb = SM.h[:, :].unsqueeze(2).broadcast_to([128, NG, m])
                lo_c, lo_s = Ct.h[:, :, 0:m], St.h[:, :, 0:m]
                tt("dve", TA.h[:, :, 0:m], lo_c, cb, ALU.mult, [Ct.key, CM.key], [TA.key])
                tt("dve", TB.h[:, :, 0:m], lo_s, sbb, ALU.mult, [St.key, SM.key], [TB.key])
                tt("dve", Ct.h[:, :, m:2 * m], TA.h[:, :, 0:m], TB.h[:, :, 0:m], ALU.subtract, [TA.key, TB.key], [Ct.key])
                tt("dve", TA.h[:, :, 0:m], lo_c, sbb, ALU.mult, [Ct.key, SM.key], [TA.key])
                tt("dve", TB.h[:, :, 0:m], lo_s, cb, ALU.mult, [St.key, CM.key], [TB.key])
                tt("dve", St.h[:, :, m:2 * m], TA.h[:, :, 0:m], TB.h[:, :, 0:m], ALU.add, [TA.key, TB.key], [St.key])
                dbl(CM, SM)
                m *= 2
            P.op("dve", lambda e: e.tensor_copy(out=f(CP), in_=f(CM)), [CM.key], [CP.key])
            P.op("dve", lambda e: e.tensor_copy(out=f(SP), in_=f(SM)), [SM.key], [SP.key])
            BR = sb("BR", [128, 24, 16], F32, ph)
            BI = sb("BI", [128, 24, 16], F32, ph)
            P.dma("sp", BR.h[:, :, :], w["s5_b_re"].ap()[l].rearrange("(gp g2) s c -> (g2 s) gp c", g2=2), [], [BR.key], BR.g, slow=True)
            P.dma("sp", BI.h[:, :, :], w["s5_b_im"].ap()[l].rearrange("(gp g2) s c -> (g2 s) gp c", g2=2), [], [BI.key], BI.g, slow=True)
            EB = sb("EB", [128, 48, 128], F32, ph)
            T1 = sb("T1", [128, 24, 16], F32, ph)
            T2 = sb("T2", [128, 24, 16], F32, ph)
            BB = sb("BB", [128, 48, 16], F32, ph)
            for ri in range(2):
                for z in range(2):
                    zrb = ZR.h[:, z * 24:(z + 1) * 24].unsqueeze(2).broadcast_to([128, 24, 16])
                    zib = ZI.h[:, z * 24:(z + 1) * 24].unsqueeze(2).broadcast_to([128, 24, 16])
                    if ri == 0:
                        tt("dve", T1.h[:, :, :], BR.h[:, :, :], zrb, ALU.mult, [BR.key, ZR.key], [T1.key])
                        tt("dve", T2.h[:, :, :], BI.h[:, :, :], zib, ALU.mult, [BI.key, ZI.key], [T2.key])
                        tt("dve", BB.h[:, z * 24:(z + 1) * 24, :], T1.h[:, :, :], T2.h[:, :, :], ALU.subtract, [T1.key, T2.key], [BB.key])
                    else:
                        tt("dve", T1.h[:, :, :], BI.h[:, :, :], zrb, ALU.mult, [BI.key, ZR.key], [T1.key])
                        tt("dve", T2.h[:, :, :], BR.h[:, :, :], zib, ALU.mult, [BR.key, ZI.key], [T2.key])
                        tt("dve", BB.h[:, z * 24:(z + 1) * 24, :], T1.h[:, :, :], T2.h[:, :, :], ALU.add, [T1.key, T2.key], [BB.key])
                P.op("dve", lambda e: e.memset(EB.h[:, :, :], 0.0), [], [EB.key])
                for z in range(2):
                    for g2 in range(2):
                        for b4 in range(4):
                            dst = EB.h[g2 * 64:(g2 + 1) * 64, z * 24 + b4:(z + 1) * 24:4, 32 * b4 + 16 * g2:32 * b4 + 16 * g2 + 16]
                            srcv = BB.h[g2 * 64:(g2 + 1) * 64, z * 24 + b4:(z + 1) * 24:4, :]
                            P.op("dve", lambda e, dst=dst, srcv=srcv: e.tensor_copy(out=dst, in_=srcv), [BB.key], [EB.key])
                for q in range(12):
                    ps = psb[q % 7]

                    def tr(e, ps=ps, q=q):
                        for c in range(4):
                            ins = e.transpose(out=ps.h[:, c * 128:(c + 1) * 128], in_=EB.h[:, q * 4 + c, :], identity=ident)
                        return ins

                    P.op("pe", tr, [EB.key, "cst"], [ps.key])
                    P.op("act", lambda e, ps=ps, q=q, ri=ri: e.activation(out=Bm.h[:, (q * 4) * 2 + ri:(q * 4 + 4) * 2:2, :],
                                                                          in_=ps.h[:, :].rearrange("p (c k) -> p c k", k=128), func=AF.Copy), [ps.key], [Bm.key])
            CB = V(EB.h[0:32, :, :], EB.key, "g_CB")
            for ri in range(2):
                P.op("dve", lambda e: e.memset(CB.h[:, :, :], 0.0), [], [CB.key])
                wc = w["s5_c_re"] if ri == 0 else w["s5_c_im"]
                for z in range(2):
                    for g2 in range(2):
                        base = ((l * 2 + z) * 48 + g2) * 16 * 64
                        src = bass.AP(wc, base, [[64, 16], [2048, 24], [1, 64]])
                        P.dma("sp", CB.h[g2 * 16:(g2 + 1) * 16, z * 24:(z + 1) * 24, g2 * 64:(g2 + 1) * 64], src, [CB.key], [CB.key], "g_CB")
                for q in range(12):
                    ps = psb[q % 7]

                    def pm(e, ps=ps, q=q):
                        for c in range(4):
                            col = q * 4 + c
                            off = 32 * (col % 4)
                            ins = e.matmul(ps.h[:, c * 128:(c + 1) * 128], lhsT=CB.h[:, col, :], rhs=cst.h[0:32, 448 + 96 - off:448 + 96 - off + 128],
                                           start=True, stop=True)
                        return ins

                    P.op("pe", pm, [CB.key, "cst"], [ps.key])
                    P.op("act", lambda e, ps=ps, q=q, ri=ri: e.activation(out=Cm.h[:, (q * 4) * 2 + ri:(q * 4 + 4) * 2:2, :],
                                                                          in_=ps.h[:, :].rearrange("p (c k) -> p c k", k=128), func=AF.Copy,
                                                                          scale=(1.0 if ri == 0 else -1.0)), [ps.key], [Cm.key])
            P.emit()

        NK = T // L5
        with ExitStack() as ph:
            su = sb("su", [128, 6, T], F32, ph)
            sub = sb("sub", [128, 6, T], BF16, ph)
            GL = sb("GL", [128, NG, 2], F32, ph)
            IN = sb("IN", [128, NG, 2], F32, ph)
            TT = sb("TT", [128, NG, 2], F32, ph)
            ybw = sb("ybw", [128, 6, T], F32, ph)
            yfw = sb("yfw", [128, 6, T], F32, ph)
            yo = sb("yo", [128, 6, T], BF16, ph)
            CAr = Ring([sb("CA%d" % i, [128, 2, L5], F32, ph) for i in range(2)])
            SAr = Ring([sb("SA%d" % i, [128, 2, L5], F32, ph) for i in range(2)])
            GIr = Ring([sb("GI%d" % i, [128, 2, L5], F32, ph) for i in range(2)])
            GGr = Ring([sb("GG%d" % i, [128, 2, L5], F32, ph) for i in range(2)])
            CGr = Ring([sb("CG%d" % i, [128, 2, L5], F32, ph) for i in range(2)])
            SGr = Ring([sb("SG%d" % i, [128, 2, L5], F32, ph) for i in range(2)])
            HHr = Ring([sb("HH%d" % i, [128, 2, L5], BF16, ph) for i in range(3)])
            bbr_ = Ring(psb[0:3])
            ybank = [psb[3], psb[4]]
            for z in range(2):
                P.op("dve", lambda e: e.memset(IN.h[:, :, :], 0.0), [], [IN.key])
                torder = range(NT) if z == 0 else range(NT - 1, -1, -1)
                for t in torder:
                    tsl = slice(t * T, (t + 1) * T)
                    P.dma("pool", su.h[:, :, :], suT[:, :, tsl].rearrange("c p t -> p c t"), [], [su.key], su.g)
                    if z == 0:
                        P.op("act", lambda e: e.activation(out=sub.h[:, :, :], in_=su.h[:, :, :], func=AF.Copy), [su.key], [sub.key])
                    else:
                        P.dma("pool", yfw.h[:, :, :], ysT_f[:, :, tsl].rearrange("c p t -> p c t"), [("ysf", t)], [yfw.key], yfw.g)
                        P.op("act", lambda e: e.activation(out=sub.h[:, :, :], in_=su.h[:, :, ::-1], func=AF.Copy), [su.key], [sub.key])
                    if (z == 0 and t * T == HALF) or (z == 1 and (t + 1) * T == HALF):
                        P.op("dve", lambda e: e.tensor_scalar(out=IN.h[:, :, :], in0=IN.h[:, :, :], scalar1=RST.h[:, 0:1], scalar2=None, op0=ALU.mult),
                             [IN.key, "RST"], [IN.key])
                    for k in range(NK):
                        ksl = slice(k * L5, (k + 1) * L5)
                        for gp in range(24):
                            col = z * 24 + gp
                            c6 = gp // 4
                            pb = bbr_.next()

                            def bu(e, pb=pb, col=col, c6=c6, ksl=ksl):
                                e.matmul(pb.h[:, 0:L5], lhsT=Bm.h[:, col * 2, :], rhs=sub.h[:, c6, ksl], start=True, stop=True)
                                return e.matmul(pb.h[:, L5:2 * L5], lhsT=Bm.h[:, col * 2 + 1, :], rhs=sub.h[:, c6, ksl], start=True, stop=True)

                            P.op("pe", bu, [Bm.key, sub.key], [pb.key])
                            A3 = pb.h[:, 0:2 * L5].rearrange("p (r t) -> p r t", r=2)
                            cb = Ct.h[:, col:col + 1, :].broadcast_to([128, 2, L5])
                            sbb = St.h[:, col:col + 1, :].broadcast_to([128, 2, L5])
                            ca, sa, gi, gg, cg, sg, hh_ = CAr.next(), SAr.next(), GIr.next(), GGr.next(), CGr.next(), SGr.next(), HHr.next()
                            P.op("dve", lambda e, ca=ca, A3=A3, cb=cb: e.tensor_tensor(out=ca.h[:, :, :], in0=A3, in1=cb, op=ALU.mult), [pb.key, Ct.key], [ca.key])
                            P.op("dve", lambda e, sa=sa, A3=A3, sbb=sbb: e.tensor_tensor(out=sa.h[:, :, :], in0=A3, in1=sbb, op=ALU.mult), [pb.key, St.key], [sa.key])
                            P.op("pool", lambda e, ca=ca, sa=sa, gi=gi: e.tensor_tensor(out=gi.h[:, 0, :], in0=ca.h[:, 0, :], in1=sa.h[:, 1, :], op=ALU.add), [ca.key, sa.key], [gi.key])
                            P.op("pool", lambda e, ca=ca, sa=sa, gi=gi: e.tensor_tensor(out=gi.h[:, 1, :], in0=ca.h[:, 1, :], in1=sa.h[:, 0, :], op=ALU.subtract), [ca.key, sa.key], [gi.key])
                            rb = RHO.h[:, col:col + 1].broadcast_to([128, L5])
                            for r in range(2):
                                P.op("dve", lambda e, gi=gi, gg=gg, r=r, rb=rb, col=col: e.tensor_tensor_scan(
                                    out=gg.h[:, r, :], data0=rb, data1=gi.h[:, r, :], initial=IN.h[:, col, r:r + 1], op0=ALU.mult, op1=ALU.add),
                                    [gi.key, RHO.key, IN.key], [gg.key])
                            P.op("pool", lambda e, gg=gg, col=col: e.tensor_copy(out=GL.h[:, col, :], in_=gg.h[:, :, L5 - 1]), [gg.key], [GL.key])
                            P.op("dve", lambda e, cg=cg, gg=gg, cb=cb: e.tensor_tensor(out=cg.h[:, :, :], in0=gg.h[:, :, :], in1=cb, op=ALU.mult), [gg.key, Ct.key], [cg.key])
                            P.op("pool", lambda e, sg=sg, gg=gg, sbb=sbb: e.tensor_tensor(out=sg.h[:, :, :], in0=gg.h[:, :, :], in1=sbb, op=ALU.mult), [gg.key, St.key], [sg.key])
                            P.op("dve", lambda e, cg=cg, sg=sg, hh_=hh_: e.tensor_tensor(out=hh_.h[:, 0, :], in0=cg.h[:, 0, :], in1=sg.h[:, 1, :], op=ALU.subtract), [cg.key, sg.key], [hh_.key])
                            P.op("pool", lambda e, cg=cg, sg=sg, hh_=hh_: e.tensor_tensor(out=hh_.h[:, 1, :], in0=cg.h[:, 1, :], in1=sg.h[:, 0, :], op=ALU.add), [cg.key, sg.key], [hh_.key])
                            yb_ = ybank[0] if c6 < 4 else ybank[1]
                            ycol = slice((c6 % 4) * L5, (c6 % 4 + 1) * L5)
                            first = (gp % 4 == 0)
                            lastg = (gp % 4 == 3)

                            def ym(e, yb_=yb_, ycol=ycol, col=col, hh_=hh_, first=first, lastg=lastg):
                                e.matmul(yb_.h[:, ycol], lhsT=Cm.h[:, col * 2, :], rhs=hh_.h[:, 0, :], start=first, stop=False)
                                return e.matmul(yb_.h[:, ycol], lhsT=Cm.h[:, col * 2 + 1, :], rhs=hh_.h[:, 1, :], start=False, stop=lastg)

                            P.op("pe", ym, [Cm.key, hh_.key], [yb_.key])
                        zs = slice(z * 24, (z + 1) * 24)
                        cpb = CP.h[:, zs].unsqueeze(2).broadcast_to([128, 24, 2])
                        spb = SP.h[:, zs].unsqueeze(2).broadcast_to([128, 24, 2])
                        P.op("dve", lambda e, zs=zs, cpb=cpb: e.tensor_tensor(out=IN.h[:, zs, :], in0=GL.h[:, zs, :], in1=cpb, op=ALU.mult), [GL.key, CP.key, IN.key], [IN.key])
                        P.op("dve", lambda e, zs=zs, spb=spb: e.tensor_tensor(out=TT.h[:, zs, :], in0=GL.h[:, zs, :], in1=spb, op=ALU.mult), [GL.key, SP.key], [TT.key])
                        P.op("dve", lambda e, zs=zs: e.tensor_tensor(out=IN.h[:, zs, 0:1], in0=IN.h[:, zs, 0:1], in1=TT.h[:, zs, 1:2], op=ALU.subtract), [IN.key, TT.key], [IN.key])
                        P.op("dve", lambda e, zs=zs: e.tensor_tensor(out=IN.h[:, zs, 1:2], in0=IN.h[:, zs, 1:2], in1=TT.h[:, zs, 0:1], op=ALU.add), [IN.key, TT.key], [IN.key])
                        ydst = yfw if z == 0 else ybw
                        P.op("act", lambda e, ydst=ydst, ksl=ksl: e.activation(out=ydst.h[:, 0:4, ksl], in_=ybank[0].h[:, :].rearrange("p (c t) -> p c t", t=L5), func=AF.Copy),
                             [ybank[0].key], [ydst.key])
                        P.op("act", lambda e, ydst=ydst, ksl=ksl: e.activation(out=ydst.h[:, 4:6, ksl], in_=ybank[1].h[:, 0:2 * L5].rearrange("p (c t) -> p c t", t=L5), func=AF.Copy),
                             [ybank[1].key], [ydst.key])
                    if z == 0:
                        P.dma("pool", ysT_f[:, :, tsl].rearrange("c p t -> p c t"), yfw.h[:, :, :], [yfw.key], [("ysf", t)], "g_yfws")
                    else:
                        for c6 in range(6):
                            dcol = S5D.h[:, l * 6 + c6:l * 6 + c6 + 1]
                            P.op("dve", lambda e, c6=c6, dcol=dcol: e.scalar_tensor_tensor(out=yfw.h[:, c6, :], in0=su.h[:, c6, :], scalar=dcol, in1=yfw.h[:, c6, :],
                                                                                            op0=ALU.mult, op1=ALU.add), [su.key, yfw.key, "S5D"], [yfw.key])
                        P.op("pool", lambda e: e.tensor_tensor(out=yfw.h[:, :, :], in0=yfw.h[:, :, :], in1=ybw.h[:, :, ::-1], op=ALU.add), [yfw.key, ybw.key], [yfw.key])
                        P.op("act", lambda e: e.activation(out=yo.h[:, :, :], in_=yfw.h[:, :, :], func=AF.Gelu), [yfw.key], [yo.key])
                        P.dma("pool", ysT[:, :, tsl].rearrange("c p t -> p c t"), yo.h[:, :, :], [yo.key], [], yo.g)
            P.emit()
        lay5.close()

    def zero_ys():
        with ExitStack() as ph:
            zt = sb("zt", [128, 3, T], F32, ph)
            P.op("dve", lambda e: e.memset(zt.h[:, :, :], 0.0), [], [zt.key])
            ztb = zt.h[:, 0:3, :].rearrange("p c t -> p (c t)").bitcast(BF16).rearrange("p (c t) -> p c t", t=T)
            for t in range(NT):
                P.dma("pool", ysT[:, :, t * T:(t + 1) * T].rearrange("c p t -> p c t"), ztb, [zt.key], [], "g_zt2")
            P.emit()

    def mixers(l):
        hgrn_phase(l)
        if "nos5" in debug:
            zero_ys()
        else:
            s5_phase(l)

    for l in range(1 if "onelayer" in debug else DEPTH):
        lay = ExitStack()
        kmT = sb("kmT", [128, DC, 2 * NMEM], BF16, lay)
        vm = sb("vm", [128, 4, D], BF16, lay)
        with ExitStack() as ph:
            cx = tile_ctx(ph)
            mt = cx.hh
            for tb in range(4):
                a = V(cx.yb.h[:, 0:4, :].rearrange("p c t -> p (c t)"), "yb0")
                P.dma("pool", a.h, mem_in[tb // 2, (tb % 2) * 128:(tb % 2 + 1) * 128, :], [], YBK[0:4], "g_yb0")
                for q in range(4):
                    ps = cx.psr.next()

                    def tr(e, ps=ps, a=a, q=q):
                        for c in range(4):
                            ins = e.transpose(out=ps.h[:, c * 128:(c + 1) * 128], in_=a.h[:, (q * 4 + c) * 128:(q * 4 + c + 1) * 128], identity=ident)
                        return ins

                    P.op("pe", tr, YBK[0:4] + ["cst"], [ps.key])
                    P.op("dve", lambda e, ps=ps, q=q, tb=tb: e.tensor_copy(out=mt.h[:, q * 4:(q + 1) * 4, tb * 128:(tb + 1) * 128],
                                                                           in_=ps.h[:, :].rearrange("p (c t) -> p c t", t=128)), [ps.key], [mt.key])
            norm_to(cx, mt.h, [mt.key], DC, lambda c: gain(l, 6, c), cx.xn.h, [cx.xn.key])
            xap = lambda kc: cx.xn.h[:, kc, :]
            for j in range(DC):
                pk = cx.lin("xa_w_kv", l, 0, j, DC, xap, [cx.xn.key])
                P.op("act", lambda e, pk=pk, j=j: e.activation(out=kmT.h[:, j, :], in_=pk.h[:, :], func=AF.Copy), [pk.key], ["kmT"])
            for j in range(DC):
                pv_ = cx.lin("xa_w_kv", l, 0, DC + j, DC, xap, [cx.xn.key])
                vb = cx.tmpr.next()
                vbb = vb.h[:, :].bitcast(BF16)
                P.op("act", lambda e, pv_=pv_, vbb=vbb: e.activation(out=vbb[:, 0:T], in_=pv_.h[:, :], func=AF.Copy), [pv_.key], [vb.key])

                def trv(e, vbb=vbb):
                    for b in range(4):
                        ins = e.transpose(out=pst.h[:, b * 128:(b + 1) * 128], in_=vbb[:, b * 128:(b + 1) * 128], identity=identb)
                    return ins

                P.op("pe", trv, [vb.key, "cstb"], ["pst"])
                P.op("dve", lambda e, j=j: e.tensor_copy(out=vm.h[:, :, j * 128:(j + 1) * 128], in_=pst.h[:, 0:512].rearrange("p (b d) -> p b d", d=128)),
                     ["pst"], ["vm"])
            P.emit()

        with ExitStack() as ph:
            cx = tile_ctx(ph)
            hh, xn, yb = cx.hh, cx.xn, cx.yb
            cst_t = sb("cos_t", [128, T], F32, ph)
            snt_t = sb("sin_t", [128, T], F32, ph)
            sqh = sb("sqh", [128, T], BF16, ph)
            xgb = sb("xgb", [128, T], BF16, ph)
            trs = [sb("trs%d" % i, [128, 4, 128], BF16, ph) for i in range(2)]
            stI = [0]

            def stage():
                c = stI[0] % DC
                stI[0] += 1
                return V(yb.h[:, c, :], YBK[c], "g_" + YBK[c])

            def stageb():
                s = stage()
                return V(s.h.bitcast(BF16)[:, 0:T], s.key, s.g)

            for t in range(NT):
                tsl = slice(t * T, (t + 1) * T)
                load_h(cx, t)
                P.dma("pool", cst_t.h[:, :], cos_in[:, tsl], [], [cst_t.key], cst_t.g)
                P.dma("pool", snt_t.h[:, :], sin_in[:, tsl], [], [snt_t.key], snt_t.g)
                ffn(cx, l, 0, hh, 0, 1)
                store_h(cx, t)
                norm_to(cx, hh.h, [hh.key], DC, lambda c: gain(l, 2, c), xn.h, [xn.key])
                xap = lambda kc: xn.h[:, kc, :]
                for j in range(106):
                    ps = cx.lin("w_in", l, 0, j, DC, xap, [xn.key])
                    if j < OFF_AV:
                        gi = 0 if j < OFF_AK else 1
                        P.op("act", lambda e, ps=ps: e.activation(out=sqh.h[:, :], in_=ps.h[:, :], func=AF.Square), [ps.key], [sqh.key])
                        rs = rstd_from(cx, lambda c: sqh.h[:, :], [sqh.key], 1, 128.0)
                        xg = cx.tmpr.next()
                        P.op("dve", lambda e, ps=ps, rs=rs, xg=xg, gi=gi: e.scalar_tensor_tensor(
                            out=xg.h[:, :], in0=ps.h[:, :], scalar=QKG.h[:, 2 * l + gi:2 * l + gi + 1], in1=rs.h[:, :], op0=ALU.mult, op1=ALU.mult),
                            [ps.key, rs.key, "QKG"], [xg.key])
                        P.op("act", lambda e, xg=xg: e.activation(out=xgb.h[:, :], in_=xg.h[:, :], func=AF.Copy), [xg.key], [xgb.key])
                        pr = cx.psr.next()
                        P.op("pe", lambda e, pr=pr: e.matmul(pr.h[:, :], lhsT=rotTb, rhs=xgb.h[:, :], start=True, stop=True), [xgb.key, "cstb"], [pr.key])
                        t2 = cx.tmpr.next()
                        P.op("dve", lambda e, pr=pr, t2=t2: e.tensor_tensor(out=t2.h[:, :], in0=pr.h[:, :], in1=snt_t.h[:, :], op=ALU.mult),
                             [pr.key, snt_t.key], [t2.key])
                        P.op("pool", lambda e, xg=xg: e.tensor_tensor(out=xg.h[:, :], in0=xg.h[:, :], in1=cst_t.h[:, :], op=ALU.mult),
                             [xg.key, cst_t.key], [xg.key])
                        so = stageb()
                        P.op("dve", lambda e, xg=xg, t2=t2, so=so: e.tensor_tensor(out=so.h, in0=xg.h[:, :], in1=t2.h[:, :], op=ALU.add),
                             [xg.key, t2.key], [so.key])
                        dst = qT[j, :, tsl] if j < OFF_AK else kT[j - OFF_AK, :, tsl]
                        P.dma("pool", dst, so.h, [so.key], [("qk", t)], so.g)
                    elif j < OFF_HQ or (OFF_HI <= j < OFF_HG):
                        so = stageb()
                        P.op("act", lambda e, ps=ps, so=so: e.activation(out=so.h, in_=ps.h[:, :], func=AF.Copy), [ps.key], [so.key])

                        def trv(e, so=so):
                            for b in range(4):
                                ins = e.transpose(out=pst.h[:, b * 128:(b + 1) * 128], in_=so.h[:, b * 128:(b + 1) * 128], identity=identb)
                            return ins

                        P.op("pe", trv, [so.key, "cstb"], ["pst"])
                        tr_ = trs[j % 2]
                        P.op("dve", lambda e, tr_=tr_: e.tensor_copy(out=tr_.h[:, :, :], in_=pst.h[:, 0:512].rearrange("p (b d) -> p b d", d=128)),
                             ["pst"], [tr_.key])
                        if j < OFF_HQ:
                            dst = vtm[j - OFF_AV, tsl, :].rearrange("(b p) d -> p b d", p=128)
                        else:
                            hd = j - OFF_HI
                            dst = hvtm[tsl, hd * 128:(hd + 1) * 128].rearrange("(b p) d -> p b d", p=128)
                        P.dma("pool", dst, tr_.h[:, :, :], [tr_.key], [("vv", t)], tr_.g)
                    elif j < OFF_FF or (OFF_SU <= j < OFF_GT):
                        so = stage()
                        P.op("act", lambda e, ps=ps, so=so: e.activation(out=so.h, in_=ps.h[:, :], func=AF.Copy), [ps.key], [so.key])
                        dst = hqT[j - OFF_HQ, :, tsl] if j < OFF_FF else suT[j - OFF_SU, :, tsl]
                        P.dma("pool", dst, so.h, [so.key], [("misc", t)], so.g)
                    elif j < OFF_HI:
                        z = 0 if j < OFF_FB else 1
                        hd = (j - OFF_FF) % 8
                        col = (l * 2 + z) * 8 + hd
                        sg = cx.tmpr.next()
                        P.op("act", lambda e, ps=ps, sg=sg: e.activation(out=sg.h[:, :], in_=ps.h[:, :], func=AF.Sigmoid), [ps.key], [sg.key])
                        P.op("dve", lambda e, sg=sg, col=col: e.tensor_scalar(out=sg.h[:, :], in0=sg.h[:, :], scalar1=OML.h[:, col:col + 1],
                                                                              scalar2=LB.h[:, col:col + 1], op0=ALU.mult, op1=ALU.add), [sg.key, "OML", "LB"], [sg.key])
                        s1 = stage()
                        P.op("act", lambda e, sg=sg, s1=s1: e.activation(out=s1.h, in_=sg.h[:, :], func=AF.Ln), [sg.key], [s1.key])
                        P.dma("pool", lfT[z, hd, :, tsl], s1.h, [s1.key], [("misc", t)], s1.g)
                        s2 = stage()
                        P.op("dve", lambda e, sg=sg, s2=s2: e.tensor_scalar(out=s2.h, in0=sg.h[:, :], scalar1=-1.0, scalar2=1.0, op0=ALU.mult, op1=ALU.add),
                             [sg.key], [s2.key])
                        P.dma("pool", kkT[z, hd, :, tsl], s2.h, [s2.key], [("misc", t)], s2.g)
                    elif j < OFF_SU:
                        so = stage()
                        P.op("act", lambda e, ps=ps, so=so: e.activation(out=so.h, in_=ps.h[:, :], func=AF.Silu), [ps.key], [so.key])
                        P.dma("pool", sgT[j - OFF_HG, :, tsl], so.h, [so.key], [("misc", t)], so.g)
                    else:
                        so = stage()
                        P.op("act", lambda e, ps=ps, so=so: e.activation(out=so.h, in_=ps.h[:, :], func=AF.Sigmoid), [ps.key], [so.key])
                        P.dma("pool", gtT[(j - OFF_GT) // DC][(j - OFF_GT) % DC, :, tsl], so.h, [so.key], [("misc", t)], so.g)
            P.emit()

        with ExitStack() as ph:
            NB = N // 128
            kts = sb("kts", [128, N], BF16, ph)
            vts = sb("vts", [128, NB, 128], BF16, ph)
            qts = [sb("qts%d" % i, [128, T], BF16, ph) for i in range(2)]
            pts = Ring([sb("pts%d" % i, [128, T], BF16, ph) for i in range(4)])
            rinv = sb("rinv", [128, T], F32, ph)
            osb = [sb("osb%d" % i, [128, T], BF16, ph) for i in range(2)]
            scr = Ring(psb[0:5])
            pacc, psum_ = psb[5], psb[6]
            scale = 128.0 ** -0.5
            qi = 0
            for kvh in range(2):
                for c0 in range(0, N, 2048):
                    c1 = min(N, c0 + 2048)
                    P.dma("pool", kts.h[:, c0:c1], kT[kvh, :, c0:c1], [], [kts.key], kts.g)
                    P.dma("pool", vts.h[:, c0 // 128:c1 // 128, :], vtm[kvh, c0:c1, :].rearrange("(b p) d -> p b d", p=128), [], [vts.key], vts.g)
                for g4 in range(4):
                    hd = kvh * 4 + g4
                    for qt in range(NT):
                        q = qts[qi % 2]
                        ob = osb[qi % 2]
                        qi += 1
                        P.dma("pool", q.h[:, :], qT[hd, :, qt * T:(qt + 1) * T], [], [q.key], q.g)
                        qhalf = 1 if qt * T >= HALF else 0
                        for kb in range(NB):
                            khalf = 1 if kb * 128 >= HALF else 0
                            m = l * 4 + 2 * khalf + qhalf
                            ps = scr.next()
                            P.op("pe", lambda e, ps=ps, kb=kb, q=q: e.matmul(ps.h[:, :], lhsT=kts.h[:, kb * 128:(kb + 1) * 128], rhs=q.h[:, :], start=True, stop=True),
                                 [kts.key, q.key], [ps.key])
                            pt = pts.next()
                            P.op("act", lambda e, ps=ps, pt=pt, m=m: e.activation(out=pt.h[:, :], in_=ps.h[:, :], func=AF.Exp, scale=scale, bias=BM.h[:, m:m + 1]),
                                 [ps.key, "BM"], [pt.key])

                            def pv(e, pt=pt, kb=kb):
                                e.matmul(pacc.h[:, :], lhsT=vts.h[:, kb, :], rhs=pt.h[:, :], start=(kb == 0), stop=(kb == NB - 1))
                                return e.matmul(psum_.h[:, :], lhsT=onesb, rhs=pt.h[:, :], start=(kb == 0), stop=(kb == NB - 1))

                            P.op("pe", pv, [vts.key, pt.key, "cstb"], [pacc.key, psum_.key])
                        P.op("dve", lambda e: e.reciprocal(out=rinv.h[:, :], in_=psum_.h[:, :]), [psum_.key], [rinv.key])
                        P.op("dve", lambda e, ob=ob: e.tensor_tensor(out=ob.h[:, :], in0=pacc.h[:, :], in1=rinv.h[:, :], op=ALU.mult), [pacc.key, rinv.key], [ob.key])
                        P.dma("pool", yattT[hd, :, qt * T:(qt + 1) * T], ob.h[:, :], [ob.key], [], ob.g)
            P.emit()

        if "nomix" in debug:
            with ExitStack() as ph:
                zt = sb("zt", [128, 8, T], F32, ph)
                P.op("dve", lambda e: e.memset(zt.h[:, :, :], 0.0), [], [zt.key])
                ztb = zt.h[:, 0:3, :].rearrange("p c t -> p (c t)").bitcast(BF16).rearrange("p (c t) -> p c t", t=T)
                for t in range(NT):
                    P.dma("pool", ohT[:, :, t * T:(t + 1) * T].rearrange("h p t -> p h t"), zt.h[:, :, :], [zt.key], [], "g_zt")
                    P.dma("pool", ysT[:, :, t * T:(t + 1) * T].rearrange("c p t -> p c t"), ztb, [zt.key], [], "g_zt2")
                P.emit()
        else:
            mixers(l)

        with ExitStack() as ph:
            cx = tile_ctx(ph)
            hh, xn, yb, h1 = cx.hh, cx.xn, cx.yb, cx.h1
            gsl = Ring([sb("gsl%d" % i, [128, T], F32, ph) for i in range(3)])
            pTs = sb("pTs", [128, 2, 128], BF16, ph)
            pex = sb("pex", [128, NMEM], F32, ph)
            pnb = sb("pnb", [128, NMEM], BF16, ph)
            sm = sb("sm", [128, 4], F32, ph)
            last = (l == DEPTH - 1)
            for t in range(NT):
                tsl = slice(t * T, (t + 1) * T)
                half = 1 if t * T >= HALF else 0
                load_h(cx, t)
                P.dma("pool", yb.h[:, 0:8, :], ohT[:, :, tsl].rearrange("h p t -> p h t"), [], YBK[0:8], "g_yb0")
                P.dma("pool", yb.h[:, 8:16, :], sgT[:, :, tsl].rearrange("h p t -> p h t"), [], YBK[8:16], "g_yb8")
                P.op("act", lambda e: e.activation(out=xn.h[:, 0:8, :], in_=yb.h[:, 0:8, :], func=AF.Square), YBK[0:8], [xn.key])
                for hd in range(8):
                    rs = rstd_from(cx, lambda c, hd=hd: xn.h[:, hd, :], [xn.key], 1, 128.0)
                    tmp = cx.tmpr.next()
                    P.op("dve", lambda e, hd=hd, rs=rs, tmp=tmp: e.scalar_tensor_tensor(
                        out=tmp.h[:, :], in0=yb.h[:, hd, :], scalar=OG.h[:, l * 8 + hd:l * 8 + hd + 1], in1=rs.h[:, :], op0=ALU.mult, op1=ALU.mult),
                        [YBK[hd], rs.key, "OG"], [tmp.key])
                    P.op("pool", lambda e, hd=hd, tmp=tmp: e.tensor_tensor(out=h1.h[:, 8 + hd, :], in0=tmp.h[:, :], in1=yb.h[:, 8 + hd, :], op=ALU.mult),
                         [tmp.key, YBK[8 + hd]], [H1K[8 + hd]])
                P.dma("pool", h1.h[:, 16:22, :], ysT[:, :, tsl].rearrange("c p t -> p c t"), [], H1K[16:22], "g_h1s")
                ysap = lambda kc: h1.h[:, 16 + kc, :]
                for c in range(6):
                    pv_ = cx.lin("s5_w_glu", l, 0, c, 6, ysap, H1K[16:22])
                    pg_ = cx.lin("s5_w_glu", l, 0, 6 + c, 6, ysap, H1K[16:22])
                    sg = cx.tmpr.next()
                    P.op("act", lambda e, sg=sg, pg_=pg_: e.activation(out=sg.h[:, :], in_=pg_.h[:, :], func=AF.Sigmoid), [pg_.key], [sg.key])
                    P.op("dve", lambda e, sg=sg, pv_=pv_, c=c: e.tensor_tensor(out=h1.h[:, 22 + c, :], in0=sg.h[:, :], in1=pv_.h[:, :], op=ALU.mult),
                         [sg.key, pv_.key], [H1K[22 + c]])
                P.dma("pool", h1.h[:, 0:8, :], yattT[:, :, tsl].rearrange("h p t -> p h t"), [], H1K[0:8], "g_h1a")
                aap = lambda kc: h1.h[:, kc, :]
                hap = lambda kc: h1.h[:, 8 + kc, :]
                sap = lambda kc: h1.h[:, 22 + kc, :]
                for c in range(DC):
                    gts = []
                    for br in range(3):
                        gs = gsl.next()
                        P.dma("pool", gs.h[:, :], gtT[br][c, :, tsl], [], [gs.key], gs.g)
                        gts.append(gs)
                    p0 = cx.lin("w_br_att", l, 0, c, 8, aap, H1K[0:8])
                    p1 = cx.lin("w_br_hg", l, 0, c, 8, hap, H1K[8:16])
                    p2 = cx.lin("w_br_s5", l, 0, c, 6, sap, H1K[22:28])
                    m0 = cx.tmpr.next()
                    P.op("dve", lambda e, p0=p0, m0=m0, g=gts[0]: e.tensor_tensor(out=m0.h[:, :], in0=p0.h[:, :], in1=g.h[:, :], op=ALU.mult), [p0.key, gts[0].key], [m0.key])
                    P.op("dve", lambda e, p1=p1, g=gts[1]: e.tensor_tensor(out=g.h[:, :], in0=p1.h[:, :], in1=g.h[:, :], op=ALU.mult), [p1.key, gts[1].key], [gts[1].key])
                    P.op("dve", lambda e, p2=p2, g=gts[2]: e.tensor_tensor(out=g.h[:, :], in0=p2.h[:, :], in1=g.h[:, :], op=ALU.mult), [p2.key, gts[2].key], [gts[2].key])
                    P.op("pool", lambda e, m0=m0, g=gts[1]: e.tensor_tensor(out=m0.h[:, :], in0=m0.h[:, :], in1=g.h[:, :], op=ALU.add), [m0.key, gts[1].key], [m0.key])
                    P.op("pool", lambda e, m0=m0, g=gts[2], c=c: e.tensor_tensor(out=h1.h[:, 28 + c, :], in0=m0.h[:, :], in1=g.h[:, :], op=ALU.add),
                         [m0.key, gts[2].key], [H1K[28 + c]])
                map_ = lambda kc: h1.h[:, 28 + kc, :]
                for c in range(DC):
                    po = cx.lin("w_out", l, 0, c, DC, map_, H1K[28:44])
                    P.op("act", lambda e, po=po, c=c: e.activation(out=yb.h[:, c, :], in_=po.h[:, :], func=AF.Copy), [po.key], [YBK[c]])
                norm_residual(cx, lambda c: gain(l, 3, c), hh, 1.0)
                norm_to(cx, hh.h, [hh.key], DC, lambda c: gain(l, 4, c), xn.h, [xn.key])
                xap = lambda kc: xn.h[:, kc, :]
                xs = 512.0 ** -0.5
                for c in range(DC):
                    pq = cx.lin("xa_w_q", l, 0, c, DC, xap, [xn.key])
                    P.op("act", lambda e, pq=pq, c=c: e.activation(out=h1.h[:, c, :], in_=pq.h[:, :], func=AF.Copy, scale=xs), [pq.key], [H1K[c]])
                for hd in range(4):
                    for sub in range(4):
                        ps = cx.psr.next()

                        def sc(e, ps=ps, hd=hd, sub=sub, half=half):
                            for dc in range(4):
                                ins = e.matmul(ps.h[:, 0:NMEM], lhsT=h1.h[:, hd * 4 + dc, sub * 128:(sub + 1) * 128],
                                               rhs=kmT.h[:, hd * 4 + dc, half * NMEM:(half + 1) * NMEM], start=(dc == 0), stop=(dc == 3))
                            return ins

                        P.op("pe", sc, H1K[hd * 4:hd * 4 + 4] + ["kmT"], [ps.key])
                        P.op("dve", lambda e, ps=ps: e.tensor_reduce(out=sm.h[:, 0:1], in_=ps.h[:, 0:NMEM], axis=mybir.AxisListType.X, op=ALU.max, negate=True),
                             [ps.key], ["sm"])
                        P.op("act", lambda e, ps=ps: e.activation(out=pex.h[:, :], in_=ps.h[:, 0:NMEM], func=AF.Exp, bias=sm.h[:, 0:1], accum_out=sm.h[:, 1:2]),
                             [ps.key, "sm"], [pex.key, "sm"])
                        P.op("dve", lambda e: e.reciprocal(out=sm.h[:, 2:3], in_=sm.h[:, 1:2]), ["sm"], ["sm"])
                        P.op("dve", lambda e: e.tensor_scalar(out=pnb.h[:, :], in0=pex.h[:, :], scalar1=sm.h[:, 2:3], scalar2=None, op0=ALU.mult), [pex.key, "sm"], [pnb.key])

                        def trp(e):
                            for b in range(2):
                                ins = e.transpose(out=pst.h[:, b * 128:(b + 1) * 128], in_=pnb.h[:, b * 128:(b + 1) * 128], identity=identb)
                            return ins

                        P.op("pe", trp, [pnb.key, "cstb"], ["pst"])
                        P.op("dve", lambda e: e.tensor_copy(out=pTs.h[:, :, :], in_=pst.h[:, 0:256].rearrange("p (b q) -> p b q", q=128)), ["pst"], [pTs.key])
                        po = cx.psr.next()

                        def pvm(e, po=po, hd=hd, half=half):
                            for dc in range(4):
                                for b in range(2):
                                    ins = e.matmul(po.h[:, dc * 128:(dc + 1) * 128], lhsT=vm.h[:, half * 2 + b, (hd * 4 + dc) * 128:(hd * 4 + dc + 1) * 128],
                                                   rhs=pTs.h[:, b, :], start=(b == 0), stop=(b == 1))
                            return ins

                        P.op("pe", pvm, [pTs.key, "vm"], [po.key])
                        P.op("act", lambda e, po=po, hd=hd, sub=sub: e.activation(out=h1.h[:, 16 + hd * 4:16 + hd * 4 + 4, sub * 128:(sub + 1) * 128],
                                                                                  in_=po.h[:, :].rearrange("p (c q) -> p c q", q=128), func=AF.Copy),
                             [po.key], H1K[16 + hd * 4:16 + hd * 4 + 4])
                oap = lambda kc: h1.h[:, 16 + kc, :]
                for c in range(DC):
                    pxo = cx.lin("xa_w_o", l, 0, c, DC, oap, H1K[16:32])
                    P.op("act", lambda e, pxo=pxo, c=c: e.activation(out=yb.h[:, c, :], in_=pxo.h[:, :], func=AF.Copy), [pxo.key], [YBK[c]])
                norm_residual(cx, lambda c: gain(l, 5, c), hh, 1.0)
                ffn(cx, l, 1, hh, 7, 8)
                store_h(cx, t)
            P.emit()
        lay.close()

    with ExitStack() as ph:
        xi = [sb("fi%d" % i, [128, DC, 128], F32, ph) for i in range(2)]
        xo2 = [sb("fo%d" % i, [128, D], F32, ph) for i in range(2)]
        for tb in range(N // 128):
            a = xi[tb % 2]
            o = xo2[tb % 2]
            P.dma("pool", a.h[:, :, :], hT[:, :, tb * 128:(tb + 1) * 128].rearrange("c p t -> p c t"), [], [a.key], a.g)
            for q in range(4):
                ps = psb[(tb * 4 + q) % 7]

                def tr(e, ps=ps, a=a, q=q):
                    for c in range(4):
                        ins = e.transpose(out=ps.h[:, c * 128:(c + 1) * 128], in_=a.h[:, q * 4 + c, :], identity=ident)
                    return ins

                P.op("pe", tr, [a.key, "cst"], [ps.key])
                if q % 2 == 0:
                    P.op("dve", lambda e, ps=ps, o=o, q=q: e.tensor_copy(out=o.h[:, q * 512:(q + 1) * 512], in_=ps.h[:, :]), [ps.key], [o.key])
                else:
                    P.op("act", lambda e, ps=ps, o=o, q=q: e.activation(out=o.h[:, q * 512:(q + 1) * 512], in_=ps.h[:, :], func=AF.Copy), [ps.key], [o.key])
            P.dma("pool", y_out[tb * 128:(tb + 1) * 128, :], o.h[:, :], [o.key], [], o.g)
        P.emit()
    P.stack.close()
    return nc


def host_consts():
    c = np.zeros((128, 704), np.float32)
    c[:, 0:128] = np.eye(128, dtype=np.float32)
    c[:, 128:256] = 1.0
    for m in range(128):
        if (m % 64) < 32:
            c[m + 32, 256 + m] = -1.0
        else:
            c[m - 32, 256 + m] = 1.0
    for a in range(HC):
        for b in range(HC):
            c[a, 384 + b] = 1.0 if a <= b else 0.0
            c[a, 416 + b] = 1.0 if a >= b else 0.0
    for i in range(32):
        c[i, 448 + 96 + i] = 1.0
    return c


def unit_inputs(x, mem2, N, single):
    n_seq = N if single else N // 2
    t = np.arange(N) % n_seq
    row = (t // 64).astype(np.float64)
    col = (t % 64).astype(np.float64)
    inv = 10000.0 ** (-np.arange(0, 64, 2, dtype=np.float64) / 64.0)
    ang = np.zeros((128, N))
    ang[0:32] = row[None, :] * inv[:, None]
    ang[32:64] = ang[0:32]
    ang[64:96] = col[None, :] * inv[:, None]
    ang[96:128] = ang[64:96]
    maskb = np.zeros((128, 4), np.float32)
    if not single:
        maskb[:, 1] = -30000.0
        maskb[:, 2] = -30000.0
    return {
        "x": np.ascontiguousarray(x, dtype=np.float32),
        "mem": np.ascontiguousarray(mem2, dtype=np.float32),
        "cos_t": np.cos(ang).astype(np.float32),
        "sin_t": np.sin(ang).astype(np.float32),
        "maskb": maskb,
        "rst": np.full((128, 1), 1.0 if single else 0.0, np.float32),
        "consts": host_consts(),
    }


_NC_CACHE = {}
UNIT_CORES = (0, 1, 4)
N_UNIT = 16384


def kernel(**inputs):
    import os
    dbg = tuple(x for x in os.environ.get("KDEBUG", "").split(",") if x)
    N = N_UNIT
    key = (N, dbg)
    if key not in _NC_CACHE:
        _NC_CACHE[key] = build(N, debug=dbg)
    nc = _NC_CACHE[key]
    xs, xp = inputs["x_sample"], inputs["x_prompt"]
    ms, mp = inputs["mem_sample"], inputs["mem_prompt"]
    wts = {k: np.ascontiguousarray(v, dtype=np.float32) for k, v in inputs.items()
           if k not in ("x_prompt", "x_sample", "mem_prompt", "mem_sample")}
    units = {
        UNIT_CORES[0]: unit_inputs(xs[0], np.stack([ms[0], ms[0]]), N, True),
        UNIT_CORES[1]: unit_inputs(xs[1], np.stack([ms[1], ms[1]]), N, True),
        UNIT_CORES[2]: unit_inputs(np.concatenate([xp[0], xp[1]], axis=0), np.stack([mp[0], mp[1]]), N, False),
    }
    idle = unit_inputs(np.zeros((N, D), np.float32), np.zeros((2, NMEM, D), np.float32), N, True)
    in_maps = []
    for c in range(8):
        m = dict(wts)
        m.update(units.get(c, idle))
        in_maps.append(m)
    res = run_bass_kernel_spmd(nc, in_maps, core_ids=list(range(8)))
    r = res.results
    y_sample = np.stack([r[UNIT_CORES[0]]["y"], r[UNIT_CORES[1]]["y"]]).astype(np.float32)
    yp = r[UNIT_CORES[2]]["y"]
    y_prompt = np.stack([yp[:N // 2], yp[N // 2:]]).astype(np.float32)
    return (y_prompt, y_sample)
```

```python
import math
from contextlib import ExitStack

import numpy as np
import ml_dtypes

import concourse.bass as bass
import concourse.mybir as mybir
from concourse.bass_utils import run_bass_kernel_spmd

F32 = mybir.dt.float32
BF16 = mybir.dt.bfloat16
AF = mybir.ActivationFunctionType
ALU = mybir.AluOpType

D = 2048
DC = 16
DFF = 5632
FC = 44
T = 512
NMEM = 256
INW = 13568
EPS = 1e-6
DEPTH = 2
L5 = 128
HC = 32
ENG = ("pe", "act", "dve", "pool", "sp")

OFF_AQ, OFF_AK, OFF_AV, OFF_HQ, OFF_FF, OFF_FB, OFF_HI, OFF_HG, OFF_SU, OFF_GT = 0, 8, 10, 12, 20, 28, 36, 44, 52, 58


class Prog:
    def __init__(self, nc):
        self.nc = nc
        self.stack = ExitStack()
        self.sems = {}
        self.cnt = {}
        self.nblk = 0
        self.reset_phase()

    def sem(self, key):
        if key not in self.sems:
            self.sems[key] = self.stack.enter_context(self.nc.semaphore("s_" + key))
            self.cnt[key] = 0
        return self.sems[key]

    def reset_phase(self):
        self.streams = {e: [] for e in ENG}
        self.waited = {e: {} for e in ENG}
        self.lastw = {}
        self.readers = {}

    def op(self, eng, fn, reads=(), writes=(), dma=None):
        need = {}

        def req(k, v):
            if need.get(k, 0) < v:
                need[k] = v

        for b in reads:
            lw = self.lastw.get(b)
            if lw is not None:
                req(*lw)
        for b in writes:
            lw = self.lastw.get(b)
            if lw is not None:
                req(*lw)
            for k, v in self.readers.get(b, {}).items():
                req(k, v)
        st = self.streams[eng]
        wd = self.waited[eng]
        for k, v in need.items():
            if k == "pe" and eng == "pe":
                continue
            if wd.get(k, 0) < v:
                st.append(("w", k, v))
                wd[k] = v
        semk = dma if dma is not None else eng
        self.sem(semk)
        inc = 16 if dma is not None else 1
        self.cnt[semk] += inc
        val = self.cnt[semk]
        st.append(("o", fn, semk, inc))
        for b in reads:
            self.readers.setdefault(b, {})[semk] = val
        for b in writes:
            self.lastw[b] = (semk, val)
            self.readers[b] = {}

    def dma(self, q, out, in_, reads, writes, group, slow=False):
        if slow:
            fn = lambda e: e.dma_start(out=out, in_=in_, allow_slow_non_contiguous=True)
        else:
            fn = lambda e: e.dma_start(out=out, in_=in_)
        self.op(q, fn, reads, writes, dma=group)

    def emit(self):
        st = self.streams["sp"]
        wd = self.waited["sp"]
        for k, v in self.cnt.items():
            if v > 0 and wd.get(k, 0) < v:
                st.append(("w", k, v))
                wd[k] = v
        sems = self.sems
        self.nblk += 1
        with self.nc.Block() as block:
            for e, dec in (("pe", block.tensor), ("act", block.scalar), ("dve", block.vector),
                           ("pool", block.gpsimd), ("sp", block.sync)):
                items = self.streams[e]
                if not items:
                    continue

                def body(engine, items=items):
                    for it in items:
                        if it[0] == "w":
                            engine.wait_ge(sems[it[1]], it[2])
                        else:
                            it[1](engine).then_inc(sems[it[2]], it[3])

                dec(body)
        self.reset_phase()


class Ring:
    def __init__(self, items):
        self.items = items
        self.i = 0

    def next(self):
        it = self.items[self.i % len(self.items)]
        self.i += 1
        return it


class TL:
    def __init__(self, h, key):
        self.h = h
        self.key = key
        self.g = "g_" + key


def build(N, debug=()):
    NT = N // T
    HALF = N // 2
    nc = bass.Bass("TRN2", target_bir_lowering=False)
    P = Prog(nc)
    top = P.stack

    def din(name, shape, dt=F32):
        return nc.dram_tensor(name, list(shape), dt, kind="ExternalInput")

    def dscr(name, shape, dt=F32):
        kind = "ExternalOutput" if name in debug else "Internal"
        return nc.dram_tensor(name, list(shape), dt, kind=kind)

    x_in = din("x", [N, D])
    mem_in = din("mem", [2, NMEM, D])
    cos_in = din("cos_t", [128, N])
    sin_in = din("sin_t", [128, N])
    maskb_in = din("maskb", [128, 4])
    rst_in = din("rst", [128, 1])
    cst_in = din("consts", [128, 704])
    w = {}
    w["norm_gains"] = din("norm_gains", [DEPTH, 9, D])
    w["ffn_w_gu"] = din("ffn_w_gu", [DEPTH, 2, D, 2 * DFF])
    w["ffn_w_down"] = din("ffn_w_down", [DEPTH, 2, DFF, D])
    w["w_in"] = din("w_in", [DEPTH, D, INW])
    w["att_qk_gain"] = din("att_qk_gain", [DEPTH, 2, 128])
    w["hg_lb_logits"] = din("hg_lb_logits", [DEPTH, 2, 1024])
    w["hg_o_gain"] = din("hg_o_gain", [DEPTH, 1024])
    w["s5_lam_re"] = din("s5_lam_re", [DEPTH, 2, 48, 64])
    w["s5_lam_im"] = din("s5_lam_im", [DEPTH, 2, 48, 64])
    w["s5_log_dt"] = din("s5_log_dt", [DEPTH, 2, 48])
    w["s5_b_re"] = din("s5_b_re", [DEPTH, 48, 64, 16])
    w["s5_b_im"] = din("s5_b_im", [DEPTH, 48, 64, 16])
    w["s5_c_re"] = din("s5_c_re", [DEPTH, 2, 48, 16, 64])
    w["s5_c_im"] = din("s5_c_im", [DEPTH, 2, 48, 16, 64])
    w["s5_d"] = din("s5_d", [DEPTH, 768])
    w["s5_w_glu"] = din("s5_w_glu", [DEPTH, 768, 1536])
    w["w_br_att"] = din("w_br_att", [DEPTH, 1024, D])
    w["w_br_hg"] = din("w_br_hg", [DEPTH, 1024, D])
    w["w_br_s5"] = din("w_br_s5", [DEPTH, 768, D])
    w["w_out"] = din("w_out", [DEPTH, D, D])
    w["xa_w_q"] = din("xa_w_q", [DEPTH, D, D])
    w["xa_w_kv"] = din("xa_w_kv", [DEPTH, D, 2 * D])
    w["xa_w_o"] = din("xa_w_o", [DEPTH, D, D])
    y_out = nc.dram_tensor("y", [N, D], F32, kind="ExternalOutput")

    hT = dscr("hT", [DC, 128, N])
    WB = {}

    def wb_alloc(name, l, i, K, Fo):
        WB[(name, l, i)] = dscr("wb_%s_%d_%d" % (name, l, i), [Fo // 128, 128, K // 128, 128], BF16)

    for l in range(DEPTH):
        for i in range(2):
            wb_alloc("ffn_w_gu", l, i, D, 2 * DFF)
            wb_alloc("ffn_w_down", l, i, DFF, D)
        wb_alloc("w_in", l, 0, D, INW)
        wb_alloc("s5_w_glu", l, 0, 768, 1536)
        wb_alloc("w_br_att", l, 0, 1024, D)
        wb_alloc("w_br_hg", l, 0, 1024, D)
        wb_alloc("w_br_s5", l, 0, 768, D)
        wb_alloc("w_out", l, 0, D, D)
        wb_alloc("xa_w_q", l, 0, D, D)
        wb_alloc("xa_w_kv", l, 0, D, 2 * D)
        wb_alloc("xa_w_o", l, 0, D, D)

    qT = dscr("qT", [8, 128, N], BF16)
    kT = dscr("kT", [2, 128, N], BF16)
    vtm = dscr("vtm", [2, N, 128], BF16)
    hqT = dscr("hqT", [8, 128, N])
    lfT = dscr("lfT", [2, 8, 128, N])
    kkT = dscr("kkT", [2, 8, 128, N])
    hvtm = dscr("hvtm", [N, 1024], BF16)
    sgT = dscr("sgT", [8, 128, N])
    suT = dscr("suT", [6, 128, N])
    gtT = [dscr("gtT%d" % b_, [DC, 128, N]) for b_ in range(3)]
    yattT = dscr("yattT", [8, 128, N], BF16)
    ohT = dscr("ohT", [8, 128, N])
    ysT = dscr("ysT", [6, 128, N], BF16)
    ysT_f = dscr("ysT_f", [6, 128, N])

    uid = [0]

    def sb(name, shape, dt=F32, stack=None):
        uid[0] += 1
        h = (stack or top).enter_context(nc.sbuf_tensor("%s_%d" % (name, uid[0]), list(shape), dt))
        return TL(h, name)

    psb = [TL(top.enter_context(nc.psum_tensor("ps%d" % i, [128, 512], F32)), "ps%d" % i) for i in range(7)]
    pst = TL(top.enter_context(nc.psum_tensor("pst", [128, 1024], BF16)), "pst")
    cst = sb("cst", [128, 704])
    cstb = sb("cstb", [128, 704], BF16)
    G = sb("G", [128, DEPTH * 9 * DC])
    epsc = sb("epsc", [128, 1])
    ident = cst.h[:, 0:128]
    identb = cstb.h[:, 0:128]
    onesb = cstb.h[:, 128:256]
    rotTb = cstb.h[:, 256:384]
    QKG = sb("QKG", [128, 4])
    BM = sb("BM", [128, 8])
    LGT = sb("LGT", [128, 32])
    LB = sb("LB", [128, 32])
    OML = sb("OML", [128, 32])
    OG = sb("OG", [128, 16])
    RST = sb("RST", [128, 1])
    MB = sb("MB", [128, 4])
    S5D = sb("S5D", [128, 12])

    def gain(l, n, c):
        o = (l * 9 + n) * DC + c
        return G.h[:, o:o + 1]

    P.dma("sp", cst.h[:, :], cst_in[:, :], [], ["cst"], cst.g)
    P.op("dve", lambda e: e.tensor_copy(out=cstb.h[:, :], in_=cst.h[:, :]), ["cst"], ["cstb"])
    P.op("dve", lambda e: e.memset(epsc.h[:, :], EPS), [], ["epsc"])
    for l in range(DEPTH):
        for n in range(9):
            o = (l * 9 + n) * DC
            P.dma("sp", G.h[:, o:o + DC], w["norm_gains"].ap()[l, n].rearrange("(c p) -> p c", p=128),
                  [], ["G"], G.g, slow=True)

    P.dma("sp", RST.h[:, :], rst_in[:, :], [], ["RST"], RST.g)
    P.dma("sp", MB.h[:, :], maskb_in[:, :], [], ["MB"], MB.g)
    for l in range(DEPTH):
        for i in range(2):
            P.dma("sp", QKG.h[:, l * 2 + i:l * 2 + i + 1], w["att_qk_gain"].ap()[l, i].rearrange("(p o) -> p o", o=1), [], ["QKG"], QKG.g, slow=True)
        for z in range(2):
            o = (l * 2 + z) * 8
            P.dma("sp", LGT.h[:, o:o + 8], w["hg_lb_logits"].ap()[l, z].rearrange("(h p) -> p h", p=128), [], ["LGT"], LGT.g, slow=True)
        P.dma("sp", OG.h[:, l * 8:(l + 1) * 8], w["hg_o_gain"].ap()[l].rearrange("(h p) -> p h", p=128), [], ["OG"], OG.g, slow=True)
        P.dma("sp", S5D.h[:, l * 6:(l + 1) * 6], w["s5_d"].ap()[l].rearrange("(c p) -> p c", p=128), [], ["S5D"], S5D.g, slow=True)
    P.op("dve", lambda e: e.memset(LB.h[:, 0:16], 0.0), [], ["LB"])
    P.op("dve", lambda e: e.tensor_tensor(out=LB.h[:, 16:32], in0=LGT.h[:, 16:32], in1=LGT.h[:, 0:16], op=ALU.subtract), ["LGT", "LB"], ["LB"])
    P.op("act", lambda e: e.activation(out=LB.h[:, 16:32], in_=LB.h[:, 16:32], func=AF.Sigmoid), ["LB"], ["LB"])
    P.op("dve", lambda e: e.tensor_scalar(out=OML.h[:, :], in0=LB.h[:, :], scalar1=-1.0, scalar2=1.0, op0=ALU.mult, op1=ALU.add), ["LB"], ["OML"])
    for l in range(DEPTH):
        pv = sb("pv%d" % l, [128, 1])
        pm = sb("pm%d" % l, [1, 2])
        P.op("dve", lambda e, l=l, pv=pv: e.tensor_tensor(out=pv.h[:, :], in0=QKG.h[:, 2 * l:2 * l + 1], in1=QKG.h[:, 2 * l + 1:2 * l + 2], op=ALU.mult), ["QKG"], [pv.key])
        P.op("pe", lambda e, pv=pv: e.transpose(out=psb[0].h[0:1, 0:128], in_=pv.h[:, 0:1], identity=ident), [pv.key, "cst"], ["ps0"])
        P.op("dve", lambda e, pm=pm: e.tensor_reduce(out=pm.h[0:1, 0:1], in_=psb[0].h[0:1, 0:128], axis=mybir.AxisListType.X, op=ALU.max, apply_absolute_value=True), ["ps0"], [pm.key])
        P.op("pe", lambda e, pm=pm: e.matmul(psb[1].h[:, 0:1], lhsT=cst.h[0:1, 128:256], rhs=pm.h[0:1, 0:1], start=True, stop=True), [pm.key, "cst"], ["ps1"])
        P.op("dve", lambda e, pv=pv: e.tensor_scalar(out=pv.h[:, :], in0=psb[1].h[:, 0:1], scalar1=-math.sqrt(128.0), scalar2=-0.5, op0=ALU.mult, op1=ALU.add), ["ps1"], [pv.key])
        P.op("dve", lambda e, l=l, pv=pv: e.tensor_scalar(out=BM.h[:, 4 * l:4 * l + 4], in0=MB.h[:, :], scalar1=pv.h[:, 0:1], scalar2=None, op0=ALU.add), [pv.key, "MB"], ["BM"])

    with ExitStack() as ph:
        stg = [sb("stg%d" % i, [128, 8, 256], F32, ph) for i in range(3)]
        stb = [sb("stb%d" % i, [128, 2, 8, 128], BF16, ph) for i in range(3)]
        ring_i = [0]
        cast_eng = Ring(["dve", "act", "dve", "pool"])

        def convert(name, l, i, K, Fo):
            src = w[name].ap()[l, i] if len(w[name].shape) == 4 else w[name].ap()[l]
            dst = WB[(name, l, i)]
            KC = K // 128
            for s in range(Fo // 256):
                for kc0 in range(0, KC, 8):
                    kg = min(8, KC - kc0)
                    a = stg[ring_i[0] % 3]
                    b = stb[ring_i[0] % 3]
                    ring_i[0] += 1
                    P.dma("sp", a.h[:, :kg, :],
                          src[kc0 * 128:(kc0 + kg) * 128, s * 256:(s + 1) * 256].rearrange("(kc p) c -> p kc c", p=128),
                          [], [a.key], a.g)
                    ce = cast_eng.next()
                    iv = a.h[:, :kg, :].rearrange("p kc (j c) -> p j kc c", c=128)
                    ov = b.h[:, :, :kg, :]
                    if ce == "act":
                        P.op("act", lambda e, ov=ov, iv=iv: e.activation(out=ov, in_=iv, func=AF.Copy), [a.key], [b.key])
                    else:
                        P.op(ce, lambda e, ov=ov, iv=iv: e.tensor_copy(out=ov, in_=iv), [a.key], [b.key])
                    P.dma("act", dst[2 * s:2 * s + 2, :, kc0:kc0 + kg, :].rearrange("j p kc c -> p j kc c"), ov,
                          [b.key], [], b.g)

        for l in range(DEPTH):
            for i in range(2):
                convert("ffn_w_gu", l, i, D, 2 * DFF)
                convert("ffn_w_down", l, i, DFF, D)
            convert("w_in", l, 0, D, INW)
            convert("s5_w_glu", l, 0, 768, 1536)
            convert("w_br_att", l, 0, 1024, D)
            convert("w_br_hg", l, 0, 1024, D)
            convert("w_br_s5", l, 0, 768, D)
            convert("w_out", l, 0, D, D)
            convert("xa_w_q", l, 0, D, D)
            convert("xa_w_kv", l, 0, D, 2 * D)
            convert("xa_w_o", l, 0, D, D)

        xs = [sb("xs%d" % i, [128, D], F32, ph) for i in range(2)]
        xo = [sb("xo%d" % i, [128, DC, 128], F32, ph) for i in range(2)]
        for tb in range(N // 128):
            a = xs[tb % 2]
            o = xo[tb % 2]
            P.dma("pool", a.h[:, :], x_in[tb * 128:(tb + 1) * 128, :], [], [a.key], a.g)
            for q in range(4):
                ps = psb[(tb * 4 + q) % 7]

                def tr(e, ps=ps, a=a, q=q):
                    for c in range(4):
                        ins = e.transpose(out=ps.h[:, c * 128:(c + 1) * 128], in_=a.h[:, (q * 4 + c) * 128:(q * 4 + c + 1) * 128],
                                          identity=ident)
                    return ins

                P.op("pe", tr, [a.key, "cst"], [ps.key])
                P.op("dve" if q % 2 == 0 else "act",
                     (lambda e, ps=ps, o=o, q=q: e.tensor_copy(out=o.h[:, q * 4:(q + 1) * 4, :], in_=ps.h[:, :].rearrange("p (c t) -> p c t", t=128)))
                     if q % 2 == 0 else
                     (lambda e, ps=ps, o=o, q=q: e.activation(out=o.h[:, q * 4:(q + 1) * 4, :], in_=ps.h[:, :].rearrange("p (c t) -> p c t", t=128), func=AF.Copy)),
                     [ps.key], [o.key])
            P.dma("pool", hT[:, :, tb * 128:(tb + 1) * 128].rearrange("c p t -> p c t"), o.h[:, :, :], [o.key], [], o.g)
        P.emit()

    class Ctx:
        pass

    class V:
        def __init__(self, ap, key, g=None):
            self.h = ap
            self.key = key
            self.g = g or ("g_" + key)

    H1K = ["h1_%d" % j for j in range(FC)]
    YBK = ["yb%d" % c for c in range(DC)]

    def make_linear(cx):
        def linear_chunk(name, l, i, j, KC, xap, xkeys, ncols=T):
            Wd = WB[(name, l, i)]
            pieces = []
            for k0 in range(0, KC, 22):
                kg = min(22, KC - k0)
                s = cx.wr.next()
                P.dma("sp", s.h[:, :kg, :], Wd[j, :, k0:k0 + kg, :], [], [s.key], s.g)
                pieces.append((s, k0, kg))
            ps = cx.psr.next()

            def mm(e):
                ins = None
                for (s, k0, kg) in pieces:
                    for kk in range(kg):
                        kc = k0 + kk
                        ins = e.matmul(ps.h[:, 0:ncols], lhsT=s.h[:, kk, :], rhs=xap(kc), start=(kc == 0), stop=(kc == KC - 1))
                return ins

            P.op("pe", mm, [s.key for (s, _, _) in pieces] + list(xkeys), [ps.key])
            return ps

        return linear_chunk

    def rstd_from(cx, sqap, sqkeys, C, Dn, wgt=1.0, ncols=T):
        ps = cx.psr.next()

        def mm(e):
            for c in range(C):
                ins = e.matmul(ps.h[:, 0:ncols], lhsT=onesb, rhs=sqap(c), start=(c == 0), stop=(c == C - 1))
            return ins

        P.op("pe", mm, list(sqkeys) + ["cstb"], [ps.key])
        rs = cx.rsr.next()
        P.op("act", lambda e: e.activation(out=rs.h[:, 0:ncols], in_=ps.h[:, 0:ncols], func=AF.Sqrt, scale=1.0 / Dn, bias=epsc.h[:, 0:1]),
             [ps.key, "epsc"], [rs.key])
        P.op("dve", lambda e: e.reciprocal(out=rs.h[:, 0:ncols], in_=rs.h[:, 0:ncols]), [rs.key], [rs.key])
        if wgt != 1.0:
            P.op("dve", lambda e: e.tensor_scalar(out=rs.h[:, 0:ncols], in0=rs.h[:, 0:ncols], scalar1=float(wgt), scalar2=None, op0=ALU.mult),
                 [rs.key], [rs.key])
        return rs

    def rms_stats(cx, src, srckeys, C, Dn, wgt=1.0, ncols=T):
        sq = cx.h1
        P.op("act", lambda e: e.activation(out=sq.h[:, :C, 0:ncols], in_=src[:, :C, 0:ncols], func=AF.Square), list(srckeys), H1K[:C])
        return rstd_from(cx, lambda c: sq.h[:, c, 0:ncols], H1K[:C], C, Dn, wgt, ncols)

    def norm_to(cx, src, srckeys, C, gfn, dst, dstkeys, Dn=D, ncols=T):
        rs = rms_stats(cx, src, srckeys, C, Dn, 1.0, ncols)
        for c in range(C):
            P.op("dve", lambda e, c=c: e.scalar_tensor_tensor(out=dst[:, c, 0:ncols], in0=src[:, c, 0:ncols], scalar=gfn(c), in1=rs.h[:, 0:ncols],
                                                               op0=ALU.mult, op1=ALU.mult), list(srckeys) + [rs.key, "G"], list(dstkeys))

    def norm_residual(cx, gfn, hh, wgt):
        yb = cx.yb
        rs = rms_stats(cx, yb.h, YBK, DC, D, wgt)
        for c in range(DC):
            tmp = cx.tmpr.next()
            P.op("dve", lambda e, c=c, tmp=tmp: e.scalar_tensor_tensor(out=tmp.h[:, :], in0=yb.h[:, c, :], scalar=gfn(c), in1=rs.h[:, :],
                                                                        op0=ALU.mult, op1=ALU.mult), [YBK[c], rs.key, "G"], [tmp.key])
            P.op("pool", lambda e, c=c, tmp=tmp: e.tensor_tensor(out=hh.h[:, c, :], in0=tmp.h[:, :], in1=hh.h[:, c, :], op=ALU.add),
                 [tmp.key, hh.key], [hh.key])

    def ffn(cx, l, i, hh, n_pre, n_post):
        lin = cx.lin
        xn, h1, yb = cx.xn, cx.h1, cx.yb
        norm_to(cx, hh.h, [hh.key], DC, lambda c: gain(l, n_pre, c), xn.h, [xn.key])
        xap = lambda kc: xn.h[:, kc, :]
        for j in range(FC):
            pg = lin("ffn_w_gu", l, i, j, DC, xap, [xn.key])
            pu = lin("ffn_w_gu", l, i, FC + j, DC, xap, [xn.key])
            sg = cx.tmpr.next()
            P.op("act", lambda e, sg=sg, pg=pg: e.activation(out=sg.h[:, :], in_=pg.h[:, :], func=AF.Silu), [pg.key], [sg.key])
            P.op("dve", lambda e, sg=sg, pu=pu, j=j: e.tensor_tensor(out=h1.h[:, j, :], in0=sg.h[:, :], in1=pu.h[:, :], op=ALU.mult),
                 [sg.key, pu.key], [H1K[j]])
        hap = lambda kc: h1.h[:, kc, :]
        for c in range(DC):
            pd = lin("ffn_w_down", l, i, c, FC, hap, H1K)
            P.op("act", lambda e, pd=pd, c=c: e.activation(out=yb.h[:, c, :], in_=pd.h[:, :], func=AF.Copy), [pd.key], [YBK[c]])
        norm_residual(cx, lambda c: gain(l, n_post, c), hh, 0.5)

    def tile_ctx(ph):
        cx = Ctx()
        cx.wr = Ring([sb("wsl%d" % i, [128, 22, 128], BF16, ph) for i in range(5)])
        cx.psr = Ring(psb)
        cx.rsr = Ring([sb("rs%d" % i, [128, T], F32, ph) for i in range(2)])
        cx.tmpr = Ring([sb("tmp%d" % i, [128, T], F32, ph) for i in range(3)])
        cx.xn = sb("xn", [128, DC, T], BF16, ph)
        cx.h1 = sb("h1", [128, FC, T], BF16, ph)
        cx.yb = sb("yb", [128, DC, T], F32, ph)
        cx.lin = make_linear(cx)
        cx.hh = sb("hh", [128, DC, T], F32, ph)
        return cx

    def load_h(cx, t):
        P.dma("pool", cx.hh.h[:, :, :], hT[:, :, t * T:(t + 1) * T].rearrange("c p t -> p c t"), [("hT", t)], [cx.hh.key], "g_hh")

    def store_h(cx, t):
        P.dma("pool", hT[:, :, t * T:(t + 1) * T].rearrange("c p t -> p c t"), cx.hh.h[:, :, :], [cx.hh.key], [("hT", t)], "g_hhs")

    def hgrn_phase(l):
        NCH = T // HC
        with ExitStack() as ph:
            smask = sb("smask", [128, T], F32, ph)
            P.op("dve", lambda e: e.memset(smask.h[:, :], 1.0), [], [smask.key])
            P.op("dve", lambda e: e.memset(smask.h[:, :].rearrange("p (c s) -> p c s", s=HC)[:, :, 0:1], 0.0), [smask.key], [smask.key])
            tri = [cst.h[0:HC, 384:384 + HC], cst.h[0:HC, 416:416 + HC]]
            HG = 2
            trb = [pst.h, psb[6].h[:, :].bitcast(BF16)]
            trk = ["pst", "ps6"]
            S = [sb("S%d" % i, [128, 128], F32, ph) for i in range(HG)]
            Sb = [sb("Sb%d" % i, [128, 128], BF16, ph) for i in range(HG)]
            bufs = []
            for i in range(HG):
                b = Ctx()
                b.lf = sb("lf%d" % i, [128, T], F32, ph)
                b.kk = sb("kk%d" % i, [128, T], F32, ph)
                b.q = sb("q%d" % i, [128, T], F32, ph)
                b.pc = sb("pc%d" % i, [128, T], F32, ph)
                b.e1 = sb("e1%d" % i, [128, T], F32, ph)
                b.e2 = sb("e2%d" % i, [128, T], F32, ph)
                b.qd = sb("qd%d" % i, [128, T], BF16, ph)
                b.kd = sb("kd%d" % i, [128, T], BF16, ph)
                b.v = sb("v%d" % i, [HC, NCH, 128], BF16, ph)
                b.ke = Ring([sb("ke%d_%d" % (i, r), [128, HC], BF16, ph) for r in range(2)])
                b.kt = Ring([sb("kt%d_%d" % (i, r), [HC, 128], BF16, ph) for r in range(2)])
                b.sc = Ring([sb("sc%d_%d" % (i, r), [HC, HC], BF16, ph) for r in range(2)])
                b.of = sb("of%d" % i, [128, T], F32, ph)
                b.ob = sb("ob%d" % i, [128, T], F32, ph)
                bufs.append(b)
            for z in range(2):
                for hg in range(8 // HG):
                    for i in range(HG):
                        P.op("dve", lambda e, i=i: e.memset(S[i].h[:, :], 0.0), [], [S[i].key])
                        P.op("pool", lambda e, i=i: e.memset(Sb[i].h[:, :], 0.0), [], [Sb[i].key])
                    torder = range(NT) if z == 0 else range(NT - 1, -1, -1)
                    for t in torder:
                        tsl = slice(t * T, (t + 1) * T)
                        if (z == 0 and t * T == HALF) or (z == 1 and (t + 1) * T == HALF):
                            for i in range(HG):
                                P.op("dve", lambda e, i=i: e.tensor_scalar(out=S[i].h[:, :], in0=S[i].h[:, :], scalar1=RST.h[:, 0:1], scalar2=None, op0=ALU.mult),
                                     [S[i].key, "RST"], [S[i].key])
                                P.op("act", lambda e, i=i: e.activation(out=Sb[i].h[:, :], in_=S[i].h[:, :], func=AF.Copy), [S[i].key], [Sb[i].key])
                        for i in range(HG):
                            b = bufs[i]
                            hd = hg * HG + i
                            P.dma("pool", b.lf.h[:, :], lfT[z, hd, :, tsl], [], [b.lf.key], b.lf.g)
                            P.dma("pool", b.kk.h[:, :], kkT[z, hd, :, tsl], [], [b.kk.key], b.kk.g)
                            P.dma("pool", b.q.h[:, :], hqT[hd, :, tsl], [], [b.q.key], b.q.g)
                            P.dma("pool", b.v.h[:, :, :], hvtm[tsl, hd * 128:(hd + 1) * 128].rearrange("(c s) d -> s c d", s=HC), [], [b.v.key], b.v.g)
                            if z == 1:
                                P.dma("pool", b.of.h[:, :], ohT[hd, :, tsl], [("ohT", hd, t)], [b.of.key], b.of.g)
                            P.op("dve", lambda e, b=b: e.tensor_tensor_scan(out=b.pc.h[:, :], data0=smask.h[:, :], data1=b.lf.h[:, :], initial=0.0,
                                                                           op0=ALU.mult, op1=ALU.add), [smask.key, b.lf.key], [b.pc.key])
                            if z == 1:
                                P.op("pool", lambda e, b=b: e.tensor_tensor(out=b.e2.h[:, :], in0=b.lf.h[:, :], in1=b.pc.h[:, :], op=ALU.subtract),
                                     [b.lf.key, b.pc.key], [b.e2.key])
                                P.op("dve", lambda e, b=b: e.tensor_tensor(
                                    out=b.lf.h[:, :].rearrange("p (c s) -> p c s", s=HC), in0=b.e2.h[:, :].rearrange("p (c s) -> p c s", s=HC),
                                    in1=b.pc.h[:, :].rearrange("p (c s) -> p c s", s=HC)[:, :, HC - 1:HC].broadcast_to([128, NCH, HC]), op=ALU.add),
                                    [b.e2.key, b.pc.key], [b.lf.key])
                            cum = b.pc if z == 0 else b.lf
                            P.op("act", lambda e, b=b, cum=cum: e.activation(out=b.e1.h[:, :], in_=cum.h[:, :], func=AF.Exp), [cum.key], [b.e1.key])
                            P.op("act", lambda e, b=b, cum=cum: e.activation(out=b.e2.h[:, :], in_=cum.h[:, :], func=AF.Exp, scale=-1.0), [cum.key], [b.e2.key])
                            P.op("dve", lambda e, b=b: e.tensor_tensor(out=b.qd.h[:, :], in0=b.q.h[:, :], in1=b.e1.h[:, :], op=ALU.mult), [b.q.key, b.e1.key], [b.qd.key])
                            P.op("pool", lambda e, b=b: e.tensor_tensor(out=b.kd.h[:, :], in0=b.kk.h[:, :], in1=b.e2.h[:, :], op=ALU.mult), [b.kk.key, b.e2.key], [b.kd.key])
                        corder = range(NCH) if z == 0 else range(NCH - 1, -1, -1)
                        for c in corder:
                            csl = slice(c * HC, (c + 1) * HC)
                            epos = c * HC + (HC - 1 if z == 0 else 0)
                            for i in range(HG):
                                b = bufs[i]
                                ecol = b.e1.h[:, epos:epos + 1]
                                ke, kt, sc = b.ke.next(), b.kt.next(), b.sc.next()
                                P.op("pool", lambda e, b=b, ke=ke, ecol=ecol, csl=csl: e.tensor_scalar(out=ke.h[:, :], in0=b.kd.h[:, csl], scalar1=ecol, scalar2=None, op0=ALU.mult),
                                     [b.kd.key, b.e1.key], [ke.key])
                                tk = trk[i]
                                P.op("pe", lambda e, ke=ke, i=i: e.transpose(out=trb[i][0:HC, 0:128], in_=ke.h[:, :], identity=identb),
                                     [ke.key, "cstb"], [tk])
                                P.op("act", lambda e, kt=kt, i=i: e.activation(out=kt.h[:, :], in_=trb[i][0:HC, 0:128], func=AF.Copy), [tk], [kt.key])
                                sk = psb[2 + i].key
                                P.op("pe", lambda e, b=b, i=i, csl=csl: e.matmul(psb[2 + i].h[0:HC, 0:HC], lhsT=b.kd.h[:, csl], rhs=b.qd.h[:, csl], start=True, stop=True),
                                     [b.kd.key, b.qd.key], [sk])
                                P.op("dve", lambda e, sc=sc, i=i, trz=tri[z]: e.tensor_tensor(out=sc.h[:, :], in0=psb[2 + i].h[0:HC, 0:HC], in1=trz, op=ALU.mult),
                                     [sk, "cst"], [sc.key])
                                ok = psb[i].key

                                def om(e, b=b, i=i, sc=sc, c=c, csl=csl):
                                    e.matmul(psb[i].h[:, csl], lhsT=b.v.h[:, c, :], rhs=sc.h[:, :], start=True, stop=False)
                                    return e.matmul(psb[i].h[:, csl], lhsT=Sb[i].h[:, :], rhs=b.qd.h[:, csl], start=False, stop=True)

                                P.op("pe", om, [b.v.key, sc.key, Sb[i].key, b.qd.key], [ok])
                                dk = psb[4 + i].key
                                P.op("pe", lambda e, b=b, i=i, kt=kt, c=c: e.matmul(psb[4 + i].h[:, 0:128], lhsT=kt.h[:, :], rhs=b.v.h[:, c, :], start=True, stop=True),
                                     [kt.key, b.v.key], [dk])
                                P.op("dve", lambda e, i=i, ecol=ecol: e.scalar_tensor_tensor(out=S[i].h[:, :], in0=S[i].h[:, :], scalar=ecol,
                                                                                              in1=psb[4 + i].h[:, 0:128], op0=ALU.mult, op1=ALU.add),
                                     [S[i].key, dk, b.e1.key], [S[i].key])
                                P.op("act", lambda e, i=i: e.activation(out=Sb[i].h[:, :], in_=S[i].h[:, :], func=AF.Copy), [S[i].key], [Sb[i].key])
                        for i in range(HG):
                            b = bufs[i]
                            hd = hg * HG + i
                            ok = psb[i].key
                            if z == 0:
                                P.op("act", lambda e, b=b, i=i: e.activation(out=b.ob.h[:, :], in_=psb[i].h[:, :], func=AF.Copy), [ok], [b.ob.key])
                            else:
                                P.op("dve", lambda e, b=b, i=i: e.tensor_tensor(out=b.ob.h[:, :], in0=psb[i].h[:, :], in1=b.of.h[:, :], op=ALU.add), [ok, b.of.key], [b.ob.key])
                            P.dma("pool", ohT[hd, :, tsl], b.ob.h[:, :], [b.ob.key], [("ohT", hd, t)], b.ob.g)
            P.emit()

    def s5_phase(l):
        NG = 48
        lay5 = ExitStack()
        Bm = sb("Bm", [128, 96, 128], BF16, lay5)
        Cm = sb("Cm", [128, 96, 128], BF16, lay5)
        Ct = sb("Ct", [128, NG, L5], F32, lay5)
        St = sb("St", [128, NG, L5], F32, lay5)
        RHO = sb("RHO", [128, NG], F32, lay5)
        CP = sb("CP", [128, NG], F32, lay5)
        SP = sb("SP", [128, NG], F32, lay5)
        with ExitStack() as ph:
            def t48(name):
                return sb(name, [128, NG], F32, ph)
            LR, LI, DT, TH, X, X2, U, CS, SN, A, B_, ZR, ZI, DEN = [t48(n) for n in
                ("LR", "LI", "DT", "TH", "X", "X2", "U", "CS", "SN", "A", "B_", "ZR", "ZI", "DEN")]
            KI = sb("KI", [128, NG], mybir.dt.int32, ph)
            for z in range(2):
                P.dma("sp", LR.h[:, z * 24:(z + 1) * 24], w["s5_lam_re"].ap()[l, z].rearrange("(gp g2) s -> (g2 s) gp", g2=2), [], [LR.key], LR.g, slow=True)
                P.dma("sp", LI.h[:, z * 24:(z + 1) * 24], w["s5_lam_im"].ap()[l, z].rearrange("(gp g2) s -> (g2 s) gp", g2=2), [], [LI.key], LI.g, slow=True)
                for g2 in range(2):
                    src = bass.AP(w["s5_log_dt"], (l * 2 + z) * 48 + g2, [[0, 64], [2, 24]])
                    P.dma("sp", DT.h[g2 * 64:(g2 + 1) * 64, z * 24:(z + 1) * 24], src, [], [DT.key], DT.g, slow=True)

            def tt(eng, out, a, b, op, rk, wk):
                P.op(eng, lambda e: e.tensor_tensor(out=out, in0=a, in1=b, op=op), rk, wk)

            def ts(eng, out, a, s1, s2, op0, op1, rk, wk):
                if s2 is None:
                    P.op(eng, lambda e: e.tensor_scalar(out=out, in0=a, scalar1=s1, scalar2=None, op0=op0), rk, wk)
                else:
                    P.op(eng, lambda e: e.tensor_scalar(out=out, in0=a, scalar1=s1, scalar2=s2, op0=op0, op1=op1), rk, wk)

            def f(tl):
                return tl.h[:, :]

            P.op("act", lambda e: e.activation(out=f(DT), in_=f(DT), func=AF.Exp), [DT.key], [DT.key])
            tt("dve", f(TH), f(LI), f(DT), ALU.mult, [LI.key, DT.key], [TH.key])
            tt("dve", f(A), f(LR), f(DT), ALU.mult, [LR.key, DT.key], [A.key])
            P.op("act", lambda e: e.activation(out=f(RHO), in_=f(A), func=AF.Exp), [A.key], [RHO.key])
            ts("dve", f(X), f(TH), 1.0 / (2.0 * math.pi), None, ALU.mult, None, [TH.key], [X.key])
            P.op("dve", lambda e: e.tensor_copy(out=KI.h[:, :], in_=f(X)), [X.key], [KI.key])
            P.op("dve", lambda e: e.tensor_copy(out=f(X), in_=KI.h[:, :]), [KI.key], [X.key])
            C1, C2 = 6.28125, 2.0 * math.pi - 6.28125
            P.op("dve", lambda e: e.scalar_tensor_tensor(out=f(U), in0=f(X), scalar=-C1, in1=f(TH), op0=ALU.mult, op1=ALU.add), [X.key, TH.key], [U.key])
            P.op("dve", lambda e: e.scalar_tensor_tensor(out=f(U), in0=f(X), scalar=-C2, in1=f(U), op0=ALU.mult, op1=ALU.add), [X.key, U.key], [U.key])
            ts("dve", f(X), f(U), 0.125, None, ALU.mult, None, [U.key], [X.key])
            tt("dve", f(X2), f(X), f(X), ALU.mult, [X.key], [X2.key])
            ts("dve", f(SN), f(X2), -1.0 / 110.0, 1.0, ALU.mult, ALU.add, [X2.key], [SN.key])
            for cden in (72.0, 42.0, 20.0, 6.0):
                tt("dve", f(SN), f(SN), f(X2), ALU.mult, [SN.key, X2.key], [SN.key])
                ts("dve", f(SN), f(SN), -1.0 / cden, 1.0, ALU.mult, ALU.add, [SN.key], [SN.key])
            tt("dve", f(SN), f(SN), f(X), ALU.mult, [SN.key, X.key], [SN.key])
            ts("dve", f(CS), f(X2), -1.0 / 90.0, 1.0, ALU.mult, ALU.add, [X2.key], [CS.key])
            for cden in (56.0, 30.0, 12.0, 2.0):
                tt("dve", f(CS), f(CS), f(X2), ALU.mult, [CS.key, X2.key], [CS.key])
                ts("dve", f(CS), f(CS), -1.0 / cden, 1.0, ALU.mult, ALU.add, [CS.key], [CS.key])

            def dbl(c, s_):
                tt("dve", f(A), f(c), f(s_), ALU.mult, [c.key, s_.key], [A.key])
                tt("dve", f(B_), f(s_), f(s_), ALU.mult, [s_.key], [B_.key])
                tt("dve", f(c), f(c), f(c), ALU.mult, [c.key], [c.key])
                tt("dve", f(c), f(c), f(B_), ALU.subtract, [c.key, B_.key], [c.key])
                ts("dve", f(s_), f(A), 2.0, None, ALU.mult, None, [A.key], [s_.key])

            for _ in range(3):
                dbl(CS, SN)
            tt("dve", f(A), f(RHO), f(CS), ALU.mult, [RHO.key, CS.key], [A.key])
            ts("dve", f(A), f(A), -1.0, None, ALU.add, None, [A.key], [A.key])
            tt("dve", f(B_), f(RHO), f(SN), ALU.mult, [RHO.key, SN.key], [B_.key])
            tt("dve", f(DEN), f(LR), f(LR), ALU.mult, [LR.key], [DEN.key])
            tt("dve", f(U), f(LI), f(LI), ALU.mult, [LI.key], [U.key])
            tt("dve", f(DEN), f(DEN), f(U), ALU.add, [DEN.key, U.key], [DEN.key])
            P.op("dve", lambda e: e.reciprocal(out=f(DEN), in_=f(DEN)), [DEN.key], [DEN.key])
            tt("dve", f(ZR), f(A), f(LR), ALU.mult, [A.key, LR.key], [ZR.key])
            tt("dve", f(U), f(B_), f(LI), ALU.mult, [B_.key, LI.key], [U.key])
            tt("dve", f(ZR), f(ZR), f(U), ALU.add, [ZR.key, U.key], [ZR.key])
            tt("dve", f(ZR), f(ZR), f(DEN), ALU.mult, [ZR.key, DEN.key], [ZR.key])
            tt("dve", f(ZI), f(B_), f(LR), ALU.mult, [B_.key, LR.key], [ZI.key])
            tt("dve", f(U), f(A), f(LI), ALU.mult, [A.key, LI.key], [U.key])
            tt("dve", f(ZI), f(ZI), f(U), ALU.subtract, [ZI.key, U.key], [ZI.key])
            tt("dve", f(ZI), f(ZI), f(DEN), ALU.mult, [ZI.key, DEN.key], [ZI.key])
            CM, SM = t48("CM"), t48("SM")
            P.op("dve", lambda e: e.tensor_copy(out=f(CM), in_=f(CS)), [CS.key], [CM.key])
            P.op("dve", lambda e: e.tensor_copy(out=f(SM), in_=f(SN)), [SN.key], [SM.key])
            P.op("dve", lambda e: e.memset(Ct.h[:, :, 0:1], 1.0), [], [Ct.key])
            P.op("dve", lambda e: e.memset(St.h[:, :, 0:1], 0.0), [], [St.key])
            TA = sb("TA", [128, NG, L5 // 2], F32, ph)
            TB = sb("TB", [128, NG, L5 // 2], F32, ph)
            m = 1
            while m < L5:
                cb = CM.h[:, :].unsqueeze(2).broadcast_to([128, NG, m])
                sbb = SM.h[:, :].unsqueeze(2).broadcast_to([128, NG, m])
                lo_c, lo_s = Ct.h[:, :, 0:m], St.h[:, :, 0:m]
                tt("dve", TA.h[:, :, 0:m], lo_c, cb, ALU.mult, [Ct.key, CM.key], [TA.key])
                tt("dve", TB.h[:, :, 0:m], lo_s, sbb, ALU.mult, [St.key, SM.key], [TB.key])
                tt("dve", Ct.h[:, :, m:2 * m], TA.h[:, :, 0:m], TB.h[:, :, 0:m], ALU.subtract, [TA.key, TB.key], [Ct.key])
                tt("dve", TA.h[:, :, 0:m], lo_c, sbb, ALU.mult, [Ct.key, SM.key], [TA.key])
                tt("dve", TB.h[:, :, 0:m], lo_s, cb, ALU.mult, [St.key, CM.key], [TB.key])
                tt("dve", St.h[:, :, m:2 * m], TA.h[:, :, 0:m], TB.h[:, :, 0:m], ALU.add, [TA.key, TB.key], [St.key])
                dbl(CM, SM)
                m *= 2
            P.op("dve", lambda e: e.tensor_copy(out=f(CP), in_=f(CM)), [CM.key], [CP.key])
            P.op("dve", lambda e: e.tensor_copy(out=f(SP), in_=f(SM)), [SM.key], [SP.key])
            BR = sb("BR", [128, 24, 16], F32, ph)
            BI = sb("BI", [128, 24, 16], F32, ph)
            P.dma("sp", BR.h[:, :, :], w["s5_b_re"].ap()[l].rearrange("(gp g2) s c -> (g2 s) gp c", g2=2), [], [BR.key], BR.g, slow=True)
            P.dma("sp", BI.h[:, :, :], w["s5_b_im"].ap()[l].rearrange("(gp g2) s c -> (g2 s) gp c", g2=2), [], [BI.key], BI.g, slow=True)
            EB = sb("EB", [128, 48, 128], F32, ph)
            T1 = sb("T1", [128, 24, 16], F32, ph)
            T2 = sb("T2", [128, 24, 16], F32, ph)
            BB = sb("BB", [128, 48, 16], F32, ph)
            for ri in range(2):
                for z in range(2):
                    zrb = ZR.h[:, z * 24:(z + 1) * 24].unsqueeze(2).broadcast_to([128, 24, 16])
                    zib = ZI.h[:, z * 24:(z + 1) * 24].unsqueeze(2).broadcast_to([128, 24, 16])
                    if ri == 0:
                        tt("dve", T1.h[:, :, :], BR.h[:, :, :], zrb, ALU.mult, [BR.key, ZR.key], [T1.key])
                        tt("dve", T2.h[:, :, :], BI.h[:, :, :], zib, ALU.mult, [BI.key, ZI.key], [T2.key])
                        tt("dve", BB.h[:, z * 24:(z + 1) * 24, :], T1.h[:, :, :], T2.h[:, :, :], ALU.subtract, [T1.key, T2.key], [BB.key])
                    else:
                        tt("dve", T1.h[:, :, :], BI.h[:, :, :], zrb, ALU.mult, [BI.key, ZR.key], [T1.key])
                        tt("dve", T2.h[:, :, :], BR.h[:, :, :], zib, ALU.mult, [BR.key, ZI.key], [T2.key])
                        tt("dve", BB.h[:, z * 24:(z + 1) * 24, :], T1.h[:, :, :], T2.h[:, :, :], ALU.add, [T1.key, T2.key], [BB.key])
                P.op("dve", lambda e: e.memset(EB.h[:, :, :], 0.0), [], [EB.key])
                for z in range(2):
                    for g2 in range(2):
                        for b4 in range(4):
                            dst = EB.h[g2 * 64:(g2 + 1) * 64, z * 24 + b4:(z + 1) * 24:4, 32 * b4 + 16 * g2:32 * b4 + 16 * g2 + 16]
                            srcv = BB.h[g2 * 64:(g2 + 1) * 64, z * 24 + b4:(z + 1) * 24:4, :]
                            P.op("dve", lambda e, dst=dst, srcv=srcv: e.tensor_copy(out=dst, in_=srcv), [BB.key], [EB.key])
                for q in range(12):
                    ps = psb[q % 7]

                    def tr(e, ps=ps, q=q):
                        for c in range(4):
                            ins = e.transpose(out=ps.h[:, c * 128:(c + 1) * 128], in_=EB.h[:, q * 4 + c, :], identity=ident)
                        return ins

                    P.op("pe", tr, [EB.key, "cst"], [ps.key])
                    P.op("act", lambda e, ps=ps, q=q, ri=ri: e.activation(out=Bm.h[:, (q * 4) * 2 + ri:(q * 4 + 4) * 2:2, :],
                                                                          in_=ps.h[:, :].rearrange("p (c k) -> p c k", k=128), func=AF.Copy), [ps.key], [Bm.key])
            CB = V(EB.h[0:32, :, :], EB.key, "g_CB")
            for ri in range(2):
                P.op("dve", lambda e: e.memset(CB.h[:, :, :], 0.0), [], [CB.key])
                wc = w["s5_c_re"] if ri == 0 else w["s5_c_im"]
                for z in range(2):
                    for g2 in range(2):
                        base = ((l * 2 + z) * 48 + g2) * 16 * 64
                        src = bass.AP(wc, base, [[64, 16], [2048, 24], [1, 64]])
                        P.dma("sp", CB.h[g2 * 16:(g2 + 1) * 16, z * 24:(z + 1) * 24, g2 * 64:(g2 + 1) * 64], src, [CB.key], [CB.key], "g_CB")
                for q in range(12):
                    ps = psb[q % 7]

                    def pm(e, ps=ps, q=q):
                        for c in range(4):
                            col = q * 4 + c
                            off = 32 * (col % 4)
                            ins = e.matmul(ps.h[:, c * 128:(c + 1) * 128], lhsT=CB.h[:, col, :], rhs=cst.h[0:32, 448 + 96 - off:448 + 96 - off + 128],
                                           start=True, stop=True)
                        return ins

                    P.op("pe", pm, [CB.key, "cst"], [ps.key])
                    P.op("act", lambda e, ps=ps, q=q, ri=ri: e.activation(out=Cm.h[:, (q * 4) * 2 + ri:(q * 4 + 4) * 2:2, :],
                                                                          in_=ps.h[:, :].rearrange("p (c k) -> p c k", k=128), func=AF.Copy,
                                                                          scale=(1.0 if ri == 0 else -1.0)), [ps.key], [Cm.key])
            P.emit()

        NK = T // L5
        with ExitStack() as ph:
            su = sb("su", [128, 6, T], F32, ph)
            sub = sb("sub", [128, 6, T], BF16, ph)
            GL = sb("GL", [128, NG, 2], F32, ph)
            IN = sb("IN", [128, NG, 2], F32, ph)
            TT = sb("TT", [128, NG, 2], F32, ph)
            ybw = sb("ybw", [128, 6, T], F32, ph)
            yfw = sb("yfw", [128, 6, T], F32, ph)
            yo = sb("yo", [128, 6, T], BF16, ph)
            CAr = Ring([sb("CA%d" % i, [128, 2, L5], F32, ph) for i in range(3)])
            SAr = Ring([sb("SA%d" % i, [128, 2, L5], F32, ph) for i in range(3)])
            GIr = Ring([sb("GI%d" % i, [128, 2, L5], F32, ph) for i in range(3)])
            GGr = Ring([sb("GG%d" % i, [128, 2, L5], F32, ph) for i in range(3)])
            CGr = Ring([sb("CG%d" % i, [128, 2, L5], F32, ph) for i in range(3)])
            SGr = Ring([sb("SG%d" % i, [128, 2, L5], F32, ph) for i in range(3)])
            HHr = Ring([sb("HH%d" % i, [128, 2, L5], BF16, ph) for i in range(4)])
            bbr_ = Ring(psb[0:3])
            ybank = [psb[3], psb[4]]
            for z in range(2):
                P.op("dve", lambda e: e.memset(IN.h[:, :, :], 0.0), [], [IN.key])
                torder = range(NT) if z == 0 else range(NT - 1, -1, -1)
                for t in torder:
                    tsl = slice(t * T, (t + 1) * T)
                    P.dma("pool", su.h[:, :, :], suT[:, :, tsl].rearrange("c p t -> p c t"), [], [su.key], su.g)
                    if z == 0:
                        P.op("act", lambda e: e.activation(out=sub.h[:, :, :], in_=su.h[:, :, :], func=AF.Copy), [su.key], [sub.key])
                    else:
                        P.dma("pool", yfw.h[:, :, :], ysT_f[:, :, tsl].rearrange("c p t -> p c t"), [("ysf", t)], [yfw.key], yfw.g)
                        P.op("act", lambda e: e.activation(out=sub.h[:, :, :], in_=su.h[:, :, ::-1], func=AF.Copy), [su.key], [sub.key])
                    if (z == 0 and t * T == HALF) or (z == 1 and (t + 1) * T == HALF):
                        P.op("dve", lambda e: e.tensor_scalar(out=IN.h[:, :, :], in0=IN.h[:, :, :], scalar1=RST.h[:, 0:1], scalar2=None, op0=ALU.mult),
                             [IN.key, "RST"], [IN.key])
                    for k in range(NK):
                        ksl = slice(k * L5, (k + 1) * L5)
                        def stage_a(gp):
                            col = z * 24 + gp
                            c6 = gp // 4
                            pb = bbr_.next()

                            def bu(e, pb=pb, col=col, c6=c6, ksl=ksl):
                                e.matmul(pb.h[:, 0:L5], lhsT=Bm.h[:, col * 2, :], rhs=sub.h[:, c6, ksl], start=True, stop=True)
                                return e.matmul(pb.h[:, L5:2 * L5], lhsT=Bm.h[:, col * 2 + 1, :], rhs=sub.h[:, c6, ksl], start=True, stop=True)

                            P.op("pe", bu, [Bm.key, sub.key], [pb.key])
                            A3 = pb.h[:, 0:2 * L5].rearrange("p (r t) -> p r t", r=2)
                            cb = Ct.h[:, col:col + 1, :].broadcast_to([128, 2, L5])
                            sbb = St.h[:, col:col + 1, :].broadcast_to([128, 2, L5])
                            ca, sa, gi, gg, cg, sg, hh_ = CAr.next(), SAr.next(), GIr.next(), GGr.next(), CGr.next(), SGr.next(), HHr.next()
                            P.op("dve", lambda e, ca=ca, A3=A3, cb=cb: e.tensor_tensor(out=ca.h[:, :, :], in0=A3, in1=cb, op=ALU.mult), [pb.key, Ct.key], [ca.key])
                            P.op("dve", lambda e, sa=sa, A3=A3, sbb=sbb: e.tensor_tensor(out=sa.h[:, :, :], in0=A3, in1=sbb, op=ALU.mult), [pb.key, St.key], [sa.key])
                            P.op("dve", lambda e, ca=ca, sa=sa, gi=gi: e.tensor_tensor(out=gi.h[:, 0, :], in0=ca.h[:, 0, :], in1=sa.h[:, 1, :], op=ALU.add), [ca.key, sa.key], [gi.key])
                            P.op("pool", lambda e, ca=ca, sa=sa, gi=gi: e.tensor_tensor(out=gi.h[:, 1, :], in0=ca.h[:, 1, :], in1=sa.h[:, 0, :], op=ALU.subtract), [ca.key, sa.key], [gi.key])
                            return (col, c6, cb, sbb, gi, gg, cg, sg, hh_)

                        def stage_b(gp, st):
                            col, c6, cb, sbb, gi, gg, cg, sg, hh_ = st
                            rb = RHO.h[:, col:col + 1].broadcast_to([128, L5])
                            for r in range(2):
                                P.op("dve", lambda e, gi=gi, gg=gg, r=r, rb=rb, col=col: e.tensor_tensor_scan(
                                    out=gg.h[:, r, :], data0=rb, data1=gi.h[:, r, :], initial=IN.h[:, col, r:r + 1], op0=ALU.mult, op1=ALU.add),
                                    [gi.key, RHO.key, IN.key], [gg.key])
                            P.op("act", lambda e, gg=gg, col=col: e.activation(out=GL.h[:, col, :], in_=gg.h[:, :, L5 - 1], func=AF.Copy), [gg.key], [GL.key])
                            P.op("dve", lambda e, cg=cg, gg=gg, cb=cb: e.tensor_tensor(out=cg.h[:, :, :], in0=gg.h[:, :, :], in1=cb, op=ALU.mult), [gg.key, Ct.key], [cg.key])
                            P.op("pool", lambda e, sg=sg, gg=gg, sbb=sbb: e.tensor_tensor(out=sg.h[:, :, :], in0=gg.h[:, :, :], in1=sbb, op=ALU.mult), [gg.key, St.key], [sg.key])
                            P.op("dve", lambda e, cg=cg, sg=sg, hh_=hh_: e.tensor_tensor(out=hh_.h[:, 0, :], in0=cg.h[:, 0, :], in1=sg.h[:, 1, :], op=ALU.subtract), [cg.key, sg.key], [hh_.key])
                            P.op("pool", lambda e, cg=cg, sg=sg, hh_=hh_: e.tensor_tensor(out=hh_.h[:, 1, :], in0=cg.h[:, 1, :], in1=sg.h[:, 0, :], op=ALU.add), [cg.key, sg.key], [hh_.key])
                            yb_ = ybank[0] if c6 < 4 else ybank[1]
                            ycol = slice((c6 % 4) * L5, (c6 % 4 + 1) * L5)
                            first = (gp % 4 == 0)
                            lastg = (gp % 4 == 3)

                            def ym(e, yb_=yb_, ycol=ycol, col=col, hh_=hh_, first=first, lastg=lastg):
                                e.matmul(yb_.h[:, ycol], lhsT=Cm.h[:, col * 2, :], rhs=hh_.h[:, 0, :], start=first, stop=False)
                                return e.matmul(yb_.h[:, ycol], lhsT=Cm.h[:, col * 2 + 1, :], rhs=hh_.h[:, 1, :], start=False, stop=lastg)

                            P.op("pe", ym, [Cm.key, hh_.key], [yb_.key])

                        st_next = stage_a(0)
                        for gp in range(24):
                            st_cur = st_next
                            if gp + 1 < 24:
                                st_next = stage_a(gp + 1)
                            stage_b(gp, st_cur)
                        zs = slice(z * 24, (z + 1) * 24)
                        cpb = CP.h[:, zs].unsqueeze(2).broadcast_to([128, 24, 2])
                        spb = SP.h[:, zs].unsqueeze(2).broadcast_to([128, 24, 2])
                        P.op("dve", lambda e, zs=zs, cpb=cpb: e.tensor_tensor(out=IN.h[:, zs, :], in0=GL.h[:, zs, :], in1=cpb, op=ALU.mult), [GL.key, CP.key, IN.key], [IN.key])
                        P.op("dve", lambda e, zs=zs, spb=spb: e.tensor_tensor(out=TT.h[:, zs, :], in0=GL.h[:, zs, :], in1=spb, op=ALU.mult), [GL.key, SP.key], [TT.key])
                        P.op("dve", lambda e, zs=zs: e.tensor_tensor(out=IN.h[:, zs, 0:1], in0=IN.h[:, zs, 0:1], in1=TT.h[:, zs, 1:2], op=ALU.subtract), [IN.key, TT.key], [IN.key])
                        P.op("dve", lambda e, zs=zs: e.tensor_tensor(out=IN.h[:, zs, 1:2], in0=IN.h[:, zs, 1:2], in1=TT.h[:, zs, 0:1], op=ALU.add), [IN.key, TT.key], [IN.key])
                        ydst = yfw if z == 0 else ybw
                        P.op("act", lambda e, ydst=ydst, ksl=ksl: e.activation(out=ydst.h[:, 0:4, ksl], in_=ybank[0].h[:, :].rearrange("p (c t) -> p c t", t=L5), func=AF.Copy),
                             [ybank[0].key], [ydst.key])
                        P.op("act", lambda e, ydst=ydst, ksl=ksl: e.activation(out=ydst.h[:, 4:6, ksl], in_=ybank[1].h[:, 0:2 * L5].rearrange("p (c t) -> p c t", t=L5), func=AF.Copy),
                             [ybank[1].key], [ydst.key])
                    if z == 0:
                        P.dma("pool", ysT_f[:, :, tsl].rearrange("c p t -> p c t"), yfw.h[:, :, :], [yfw.key], [("ysf", t)], "g_yfws")
                    else:
                        for c6 in range(6):
                            dcol = S5D.h[:, l * 6 + c6:l * 6 + c6 + 1]
                            P.op("dve", lambda e, c6=c6, dcol=dcol: e.scalar_tensor_tensor(out=yfw.h[:, c6, :], in0=su.h[:, c6, :], scalar=dcol, in1=yfw.h[:, c6, :],
                                                                                            op0=ALU.mult, op1=ALU.add), [su.key, yfw.key, "S5D"], [yfw.key])
                        P.op("pool", lambda e: e.tensor_tensor(out=yfw.h[:, :, :], in0=yfw.h[:, :, :], in1=ybw.h[:, :, ::-1], op=ALU.add), [yfw.key, ybw.key], [yfw.key])
                        P.op("act", lambda e: e.activation(out=yo.h[:, :, :], in_=yfw.h[:, :, :], func=AF.Gelu), [yfw.key], [yo.key])
                        P.dma("pool", ysT[:, :, tsl].rearrange("c p t -> p c t"), yo.h[:, :, :], [yo.key], [], yo.g)
            P.emit()
        lay5.close()

    def zero_ys():
        with ExitStack() as ph:
            zt = sb("zt", [128, 3, T], F32, ph)
            P.op("dve", lambda e: e.memset(zt.h[:, :, :], 0.0), [], [zt.key])
            ztb = zt.h[:, 0:3, :].rearrange("p c t -> p (c t)").bitcast(BF16).rearrange("p (c t) -> p c t", t=T)
            for t in range(NT):
                P.dma("pool", ysT[:, :, t * T:(t + 1) * T].rearrange("c p t -> p c t"), ztb, [zt.key], [], "g_zt2")
            P.emit()

    def mixers(l):
        hgrn_phase(l)
        if "nos5" in debug:
            zero_ys()
        else:
            s5_phase(l)

    for l in range(1 if "onelayer" in debug else DEPTH):
        lay = ExitStack()
        kmT = sb("kmT", [128, DC, 2 * NMEM], BF16, lay)
        vm = sb("vm", [128, 4, D], BF16, lay)
        with ExitStack() as ph:
            cx = tile_ctx(ph)
            mt = cx.hh
            for tb in range(4):
                a = V(cx.yb.h[:, 0:4, :].rearrange("p c t -> p (c t)"), "yb0")
                P.dma("pool", a.h, mem_in[tb // 2, (tb % 2) * 128:(tb % 2 + 1) * 128, :], [], YBK[0:4], "g_yb0")
                for q in range(4):
                    ps = cx.psr.next()

                    def tr(e, ps=ps, a=a, q=q):
                        for c in range(4):
                            ins = e.transpose(out=ps.h[:, c * 128:(c + 1) * 128], in_=a.h[:, (q * 4 + c) * 128:(q * 4 + c + 1) * 128], identity=ident)
                        return ins

                    P.op("pe", tr, YBK[0:4] + ["cst"], [ps.key])
                    P.op("dve", lambda e, ps=ps, q=q, tb=tb: e.tensor_copy(out=mt.h[:, q * 4:(q + 1) * 4, tb * 128:(tb + 1) * 128],
                                                                           in_=ps.h[:, :].rearrange("p (c t) -> p c t", t=128)), [ps.key], [mt.key])
            norm_to(cx, mt.h, [mt.key], DC, lambda c: gain(l, 6, c), cx.xn.h, [cx.xn.key])
            xap = lambda kc: cx.xn.h[:, kc, :]
            for j in range(DC):
                pk = cx.lin("xa_w_kv", l, 0, j, DC, xap, [cx.xn.key])
                P.op("act", lambda e, pk=pk, j=j: e.activation(out=kmT.h[:, j, :], in_=pk.h[:, :], func=AF.Copy), [pk.key], ["kmT"])
            for j in range(DC):
                pv_ = cx.lin("xa_w_kv", l, 0, DC + j, DC, xap, [cx.xn.key])
                vb = cx.tmpr.next()
                vbb = vb.h[:, :].bitcast(BF16)
                P.op("act", lambda e, pv_=pv_, vbb=vbb: e.activation(out=vbb[:, 0:T], in_=pv_.h[:, :], func=AF.Copy), [pv_.key], [vb.key])

                def trv(e, vbb=vbb):
                    for b in range(4):
                        ins = e.transpose(out=pst.h[:, b * 128:(b + 1) * 128], in_=vbb[:, b * 128:(b + 1) * 128], identity=identb)
                    return ins

                P.op("pe", trv, [vb.key, "cstb"], ["pst"])
                P.op("dve", lambda e, j=j: e.tensor_copy(out=vm.h[:, :, j * 128:(j + 1) * 128], in_=pst.h[:, 0:512].rearrange("p (b d) -> p b d", d=128)),
                     ["pst"], ["vm"])
            P.emit()

        with ExitStack() as ph:
            cx = tile_ctx(ph)
            hh, xn, yb = cx.hh, cx.xn, cx.yb
            cst_t = sb("cos_t", [128, T], F32, ph)
            snt_t = sb("sin_t", [128, T], F32, ph)
            sqh = sb("sqh", [128, T], BF16, ph)
            xgb = sb("xgb", [128, T], BF16, ph)
            trs = [sb("trs%d" % i, [128, 4, 128], BF16, ph) for i in range(2)]
            stI = [0]

            def stage():
                c = stI[0] % DC
                stI[0] += 1
                return V(yb.h[:, c, :], YBK[c], "g_" + YBK[c])

            def stageb():
                s = stage()
                return V(s.h.bitcast(BF16)[:, 0:T], s.key, s.g)

            for t in range(NT):
                tsl = slice(t * T, (t + 1) * T)
                load_h(cx, t)
                P.dma("pool", cst_t.h[:, :], cos_in[:, tsl], [], [cst_t.key], cst_t.g)
                P.dma("pool", snt_t.h[:, :], sin_in[:, tsl], [], [snt_t.key], snt_t.g)
                ffn(cx, l, 0, hh, 0, 1)
                store_h(cx, t)
                norm_to(cx, hh.h, [hh.key], DC, lambda c: gain(l, 2, c), xn.h, [xn.key])
                xap = lambda kc: xn.h[:, kc, :]
                for j in range(106):
                    ps = cx.lin("w_in", l, 0, j, DC, xap, [xn.key])
                    if j < OFF_AV:
                        gi = 0 if j < OFF_AK else 1
                        P.op("act", lambda e, ps=ps: e.activation(out=sqh.h[:, :], in_=ps.h[:, :], func=AF.Square), [ps.key], [sqh.key])
                        rs = rstd_from(cx, lambda c: sqh.h[:, :], [sqh.key], 1, 128.0)
                        xg = cx.tmpr.next()
                        P.op("dve", lambda e, ps=ps, rs=rs, xg=xg, gi=gi: e.scalar_tensor_tensor(
                            out=xg.h[:, :], in0=ps.h[:, :], scalar=QKG.h[:, 2 * l + gi:2 * l + gi + 1], in1=rs.h[:, :], op0=ALU.mult, op1=ALU.mult),
                            [ps.key, rs.key, "QKG"], [xg.key])
                        P.op("act", lambda e, xg=xg: e.activation(out=xgb.h[:, :], in_=xg.h[:, :], func=AF.Copy), [xg.key], [xgb.key])
                        pr = cx.psr.next()
                        P.op("pe", lambda e, pr=pr: e.matmul(pr.h[:, :], lhsT=rotTb, rhs=xgb.h[:, :], start=True, stop=True), [xgb.key, "cstb"], [pr.key])
                        t2 = cx.tmpr.next()
                        P.op("dve", lambda e, pr=pr, t2=t2: e.tensor_tensor(out=t2.h[:, :], in0=pr.h[:, :], in1=snt_t.h[:, :], op=ALU.mult),
                             [pr.key, snt_t.key], [t2.key])
                        P.op("pool", lambda e, xg=xg: e.tensor_tensor(out=xg.h[:, :], in0=xg.h[:, :], in1=cst_t.h[:, :], op=ALU.mult),
                             [xg.key, cst_t.key], [xg.key])
                        so = stageb()
                        P.op("dve", lambda e, xg=xg, t2=t2, so=so: e.tensor_tensor(out=so.h, in0=xg.h[:, :], in1=t2.h[:, :], op=ALU.add),
                             [xg.key, t2.key], [so.key])
                        dst = qT[j, :, tsl] if j < OFF_AK else kT[j - OFF_AK, :, tsl]
                        P.dma("pool", dst, so.h, [so.key], [("qk", t)], so.g)
                    elif j < OFF_HQ or (OFF_HI <= j < OFF_HG):
                        so = stageb()
                        P.op("act", lambda e, ps=ps, so=so: e.activation(out=so.h, in_=ps.h[:, :], func=AF.Copy), [ps.key], [so.key])

                        def trv(e, so=so):
                            for b in range(4):
                                ins = e.transpose(out=pst.h[:, b * 128:(b + 1) * 128], in_=so.h[:, b * 128:(b + 1) * 128], identity=identb)
                            return ins

                        P.op("pe", trv, [so.key, "cstb"], ["pst"])
                        tr_ = trs[j % 2]
                        P.op("dve", lambda e, tr_=tr_: e.tensor_copy(out=tr_.h[:, :, :], in_=pst.h[:, 0:512].rearrange("p (b d) -> p b d", d=128)),
                             ["pst"], [tr_.key])
                        if j < OFF_HQ:
                            dst = vtm[j - OFF_AV, tsl, :].rearrange("(b p) d -> p b d", p=128)
                        else:
                            hd = j - OFF_HI
                            dst = hvtm[tsl, hd * 128:(hd + 1) * 128].rearrange("(b p) d -> p b d", p=128)
                        P.dma("pool", dst, tr_.h[:, :, :], [tr_.key], [("vv", t)], tr_.g)
                    elif j < OFF_FF or (OFF_SU <= j < OFF_GT):
                        so = stage()
                        P.op("act", lambda e, ps=ps, so=so: e.activation(out=so.h, in_=ps.h[:, :], func=AF.Copy), [ps.key], [so.key])
                        dst = hqT[j - OFF_HQ, :, tsl] if j < OFF_FF else suT[j - OFF_SU, :, tsl]
                        P.dma("pool", dst, so.h, [so.key], [("misc", t)], so.g)
                    elif j < OFF_HI:
                        z = 0 if j < OFF_FB else 1
                        hd = (j - OFF_FF) % 8
                        col = (l * 2 + z) * 8 + hd
                        sg = cx.tmpr.next()
                        P.op("act", lambda e, ps=ps, sg=sg: e.activation(out=sg.h[:, :], in_=ps.h[:, :], func=AF.Sigmoid), [ps.key], [sg.key])
                        P.op("dve", lambda e, sg=sg, col=col: e.tensor_scalar(out=sg.h[:, :], in0=sg.h[:, :], scalar1=OML.h[:, col:col + 1],
                                                                              scalar2=LB.h[:, col:col + 1], op0=ALU.mult, op1=ALU.add), [sg.key, "OML", "LB"], [sg.key])
                        s1 = stage()
                        P.op("act", lambda e, sg=sg, s1=s1: e.activation(out=s1.h, in_=sg.h[:, :], func=AF.Ln), [sg.key], [s1.key])
                        P.dma("pool", lfT[z, hd, :, tsl], s1.h, [s1.key], [("misc", t)], s1.g)
                        s2 = stage()
                        P.op("dve", lambda e, sg=sg, s2=s2: e.tensor_scalar(out=s2.h, in0=sg.h[:, :], scalar1=-1.0, scalar2=1.0, op0=ALU.mult, op1=ALU.add),
                             [sg.key], [s2.key])
                        P.dma("pool", kkT[z, hd, :, tsl], s2.h, [s2.key], [("misc", t)], s2.g)
                    elif j < OFF_SU:
                        so = stage()
                        P.op("act", lambda e, ps=ps, so=so: e.activation(out=so.h, in_=ps.h[:, :], func=AF.Silu), [ps.key], [so.key])
                        P.dma("pool", sgT[j - OFF_HG, :, tsl], so.h, [so.key], [("misc", t)], so.g)
                    else:
                        so = stage()
                        P.op("act", lambda e, ps=ps, so=so: e.activation(out=so.h, in_=ps.h[:, :], func=AF.Sigmoid), [ps.key], [so.key])
                        P.dma("pool", gtT[(j - OFF_GT) // DC][(j - OFF_GT) % DC, :, tsl], so.h, [so.key], [("misc", t)], so.g)
            P.emit()

        with ExitStack() as ph:
            NB = N // 128
            kts = sb("kts", [128, N], BF16, ph)
            vts = sb("vts", [128, NB, 128], BF16, ph)
            qts = [sb("qts%d" % i, [128, T], BF16, ph) for i in range(2)]
            pts = Ring([sb("pts%d" % i, [128, T], BF16, ph) for i in range(4)])
            rinv = sb("rinv", [128, T], F32, ph)
            osb = [sb("osb%d" % i, [128, T], BF16, ph) for i in range(2)]
            scr = Ring(psb[0:5])
            pacc, psum_ = psb[5], psb[6]
            scale = 128.0 ** -0.5
            qi = 0
            for kvh in range(2):
                for c0 in range(0, N, 2048):
                    c1 = min(N, c0 + 2048)
                    P.dma("pool", kts.h[:, c0:c1], kT[kvh, :, c0:c1], [], [kts.key], kts.g)
                    P.dma("pool", vts.h[:, c0 // 128:c1 // 128, :], vtm[kvh, c0:c1, :].rearrange("(b p) d -> p b d", p=128), [], [vts.key], vts.g)
                for g4 in range(4):
                    hd = kvh * 4 + g4
                    for qt in range(NT):
                        q = qts[qi % 2]
                        ob = osb[qi % 2]
                        qi += 1
                        P.dma("pool", q.h[:, :], qT[hd, :, qt * T:(qt + 1) * T], [], [q.key], q.g)
                        qhalf = 1 if qt * T >= HALF else 0
                        for kb in range(NB):
                            khalf = 1 if kb * 128 >= HALF else 0
                            m = l * 4 + 2 * khalf + qhalf
                            ps = scr.next()
                            P.op("pe", lambda e, ps=ps, kb=kb, q=q: e.matmul(ps.h[:, :], lhsT=kts.h[:, kb * 128:(kb + 1) * 128], rhs=q.h[:, :], start=True, stop=True),
                                 [kts.key, q.key], [ps.key])
                            pt = pts.next()
                            P.op("act", lambda e, ps=ps, pt=pt, m=m: e.activation(out=pt.h[:, :], in_=ps.h[:, :], func=AF.Exp, scale=scale, bias=BM.h[:, m:m + 1]),
                                 [ps.key, "BM"], [pt.key])

                            def pv(e, pt=pt, kb=kb):
                                e.matmul(pacc.h[:, :], lhsT=vts.h[:, kb, :], rhs=pt.h[:, :], start=(kb == 0), stop=(kb == NB - 1))
                                return e.matmul(psum_.h[:, :], lhsT=onesb, rhs=pt.h[:, :], start=(kb == 0), stop=(kb == NB - 1))

                            P.op("pe", pv, [vts.key, pt.key, "cstb"], [pacc.key, psum_.key])
                        P.op("dve", lambda e: e.reciprocal(out=rinv.h[:, :], in_=psum_.h[:, :]), [psum_.key], [rinv.key])
                        P.op("dve", lambda e, ob=ob: e.tensor_tensor(out=ob.h[:, :], in0=pacc.h[:, :], in1=rinv.h[:, :], op=ALU.mult), [pacc.key, rinv.key], [ob.key])
                        P.dma("pool", yattT[hd, :, qt * T:(qt + 1) * T], ob.h[:, :], [ob.key], [], ob.g)
            P.emit()

        if "nomix" in debug:
            with ExitStack() as ph:
                zt = sb("zt", [128, 8, T], F32, ph)
                P.op("dve", lambda e: e.memset(zt.h[:, :, :], 0.0), [], [zt.key])
                ztb = zt.h[:, 0:3, :].rearrange("p c t -> p (c t)").bitcast(BF16).rearrange("p (c t) -> p c t", t=T)
                for t in range(NT):
                    P.dma("pool", ohT[:, :, t * T:(t + 1) * T].rearrange("h p t -> p h t"), zt.h[:, :, :], [zt.key], [], "g_zt")
                    P.dma("pool", ysT[:, :, t * T:(t + 1) * T].rearrange("c p t -> p c t"), ztb, [zt.key], [], "g_zt2")
                P.emit()
        else:
            mixers(l)

        with ExitStack() as ph:
            cx = tile_ctx(ph)
            hh, xn, yb, h1 = cx.hh, cx.xn, cx.yb, cx.h1
            gsl = Ring([sb("gsl%d" % i, [128, T], F32, ph) for i in range(3)])
            pTs = sb("pTs", [128, 2, 128], BF16, ph)
            pex = sb("pex", [128, NMEM], F32, ph)
            pnb = sb("pnb", [128, NMEM], BF16, ph)
            sm = sb("sm", [128, 4], F32, ph)
            last = (l == DEPTH - 1)
            for t in range(NT):
                tsl = slice(t * T, (t + 1) * T)
                half = 1 if t * T >= HALF else 0
                load_h(cx, t)
                P.dma("pool", yb.h[:, 0:8, :], ohT[:, :, tsl].rearrange("h p t -> p h t"), [], YBK[0:8], "g_yb0")
                P.dma("pool", yb.h[:, 8:16, :], sgT[:, :, tsl].rearrange("h p t -> p h t"), [], YBK[8:16], "g_yb8")
                P.op("act", lambda e: e.activation(out=xn.h[:, 0:8, :], in_=yb.h[:, 0:8, :], func=AF.Square), YBK[0:8], [xn.key])
                for hd in range(8):
                    rs = rstd_from(cx, lambda c, hd=hd: xn.h[:, hd, :], [xn.key], 1, 128.0)
                    tmp = cx.tmpr.next()
                    P.op("dve", lambda e, hd=hd, rs=rs, tmp=tmp: e.scalar_tensor_tensor(
                        out=tmp.h[:, :], in0=yb.h[:, hd, :], scalar=OG.h[:, l * 8 + hd:l * 8 + hd + 1], in1=rs.h[:, :], op0=ALU.mult, op1=ALU.mult),
                        [YBK[hd], rs.key, "OG"], [tmp.key])
                    P.op("pool", lambda e, hd=hd, tmp=tmp: e.tensor_tensor(out=h1.h[:, 8 + hd, :], in0=tmp.h[:, :], in1=yb.h[:, 8 + hd, :], op=ALU.mult),
                         [tmp.key, YBK[8 + hd]], [H1K[8 + hd]])
                P.dma("pool", h1.h[:, 16:22, :], ysT[:, :, tsl].rearrange("c p t -> p c t"), [], H1K[16:22], "g_h1s")
                ysap = lambda kc: h1.h[:, 16 + kc, :]
                for c in range(6):
                    pv_ = cx.lin("s5_w_glu", l, 0, c, 6, ysap, H1K[16:22])
                    pg_ = cx.lin("s5_w_glu", l, 0, 6 + c, 6, ysap, H1K[16:22])
                    sg = cx.tmpr.next()
                    P.op("act", lambda e, sg=sg, pg_=pg_: e.activation(out=sg.h[:, :], in_=pg_.h[:, :], func=AF.Sigmoid), [pg_.key], [sg.key])
                    P.op("dve", lambda e, sg=sg, pv_=pv_, c=c: e.tensor_tensor(out=h1.h[:, 22 + c, :], in0=sg.h[:, :], in1=pv_.h[:, :], op=ALU.mult),
                         [sg.key, pv_.key], [H1K[22 + c]])
                P.dma("pool", h1.h[:, 0:8, :], yattT[:, :, tsl].rearrange("h p t -> p h t"), [], H1K[0:8], "g_h1a")
                aap = lambda kc: h1.h[:, kc, :]
                hap = lambda kc: h1.h[:, 8 + kc, :]
                sap = lambda kc: h1.h[:, 22 + kc, :]
                for c in range(DC):
                    gts = []
                    for br in range(3):
                        gs = gsl.next()
                        P.dma("pool", gs.h[:, :], gtT[br][c, :, tsl], [], [gs.key], gs.g)
                        gts.append(gs)
                    p0 = cx.lin("w_br_att", l, 0, c, 8, aap, H1K[0:8])
                    p1 = cx.lin("w_br_hg", l, 0, c, 8, hap, H1K[8:16])
                    p2 = cx.lin("w_br_s5", l, 0, c, 6, sap, H1K[22:28])
                    m0 = cx.tmpr.next()
                    P.op("dve", lambda e, p0=p0, m0=m0, g=gts[0]: e.tensor_tensor(out=m0.h[:, :], in0=p0.h[:, :], in1=g.h[:, :], op=ALU.mult), [p0.key, gts[0].key], [m0.key])
                    P.op("dve", lambda e, p1=p1, g=gts[1]: e.tensor_tensor(out=g.h[:, :], in0=p1.h[:, :], in1=g.h[:, :], op=ALU.mult), [p1.key, gts[1].key], [gts[1].key])
                    P.op("dve", lambda e, p2=p2, g=gts[2]: e.tensor_tensor(out=g.h[:, :], in0=p2.h[:, :], in1=g.h[:, :], op=ALU.mult), [p2.key, gts[2].key], [gts[2].key])
                    P.op("pool", lambda e, m0=m0, g=gts[1]: e.tensor_tensor(out=m0.h[:, :], in0=m0.h[:, :], in1=g.h[:, :], op=ALU.add), [m0.key, gts[1].key], [m0.key])
                    P.op("pool", lambda e, m0=m0, g=gts[2], c=c: e.tensor_tensor(out=h1.h[:, 28 + c, :], in0=m0.h[:, :], in1=g.h[:, :], op=ALU.add),
                         [m0.key, gts[2].key], [H1K[28 + c]])
                map_ = lambda kc: h1.h[:, 28 + kc, :]
                for c in range(DC):
                    po = cx.lin("w_out", l, 0, c, DC, map_, H1K[28:44])
                    P.op("act", lambda e, po=po, c=c: e.activation(out=yb.h[:, c, :], in_=po.h[:, :], func=AF.Copy), [po.key], [YBK[c]])
                norm_residual(cx, lambda c: gain(l, 3, c), hh, 1.0)
                norm_to(cx, hh.h, [hh.key], DC, lambda c: gain(l, 4, c), xn.h, [xn.key])
                xap = lambda kc: xn.h[:, kc, :]
                xs = 512.0 ** -0.5
                for c in range(DC):
                    pq = cx.lin("xa_w_q", l, 0, c, DC, xap, [xn.key])
                    P.op("act", lambda e, pq=pq, c=c: e.activation(out=h1.h[:, c, :], in_=pq.h[:, :], func=AF.Copy, scale=xs), [pq.key], [H1K[c]])
                for hd in range(4):
                    for sub in range(4):
                        ps = cx.psr.next()

                        def sc(e, ps=ps, hd=hd, sub=sub, half=half):
                            for dc in range(4):
                                ins = e.matmul(ps.h[:, 0:NMEM], lhsT=h1.h[:, hd * 4 + dc, sub * 128:(sub + 1) * 128],
                                               rhs=kmT.h[:, hd * 4 + dc, half * NMEM:(half + 1) * NMEM], start=(dc == 0), stop=(dc == 3))
                            return ins

                        P.op("pe", sc, H1K[hd * 4:hd * 4 + 4] + ["kmT"], [ps.key])
                        P.op("dve", lambda e, ps=ps: e.tensor_reduce(out=sm.h[:, 0:1], in_=ps.h[:, 0:NMEM], axis=mybir.AxisListType.X, op=ALU.max, negate=True),
                             [ps.key], ["sm"])
                        P.op("act", lambda e, ps=ps: e.activation(out=pex.h[:, :], in_=ps.h[:, 0:NMEM], func=AF.Exp, bias=sm.h[:, 0:1], accum_out=sm.h[:, 1:2]),
                             [ps.key, "sm"], [pex.key, "sm"])
                        P.op("dve", lambda e: e.reciprocal(out=sm.h[:, 2:3], in_=sm.h[:, 1:2]), ["sm"], ["sm"])
                        P.op("dve", lambda e: e.tensor_scalar(out=pnb.h[:, :], in0=pex.h[:, :], scalar1=sm.h[:, 2:3], scalar2=None, op0=ALU.mult), [pex.key, "sm"], [pnb.key])

                        def trp(e):
                            for b in range(2):
                                ins = e.transpose(out=pst.h[:, b * 128:(b + 1) * 128], in_=pnb.h[:, b * 128:(b + 1) * 128], identity=identb)
                            return ins

                        P.op("pe", trp, [pnb.key, "cstb"], ["pst"])
                        P.op("dve", lambda e: e.tensor_copy(out=pTs.h[:, :, :], in_=pst.h[:, 0:256].rearrange("p (b q) -> p b q", q=128)), ["pst"], [pTs.key])
                        po = cx.psr.next()

                        def pvm(e, po=po, hd=hd, half=half):
                            for dc in range(4):
                                for b in range(2):
                                    ins = e.matmul(po.h[:, dc * 128:(dc + 1) * 128], lhsT=vm.h[:, half * 2 + b, (hd * 4 + dc) * 128:(hd * 4 + dc + 1) * 128],
                                                   rhs=pTs.h[:, b, :], start=(b == 0), stop=(b == 1))
                            return ins

                        P.op("pe", pvm, [pTs.key, "vm"], [po.key])
                        P.op("act", lambda e, po=po, hd=hd, sub=sub: e.activation(out=h1.h[:, 16 + hd * 4:16 + hd * 4 + 4, sub * 128:(sub + 1) * 128],
                                                                                  in_=po.h[:, :].rearrange("p (c q) -> p c q", q=128), func=AF.Copy),
                             [po.key], H1K[16 + hd * 4:16 + hd * 4 + 4])
                oap = lambda kc: h1.h[:, 16 + kc, :]
                for c in range(DC):
                    pxo = cx.lin("xa_w_o", l, 0, c, DC, oap, H1K[16:32])
                    P.op("act", lambda e, pxo=pxo, c=c: e.activation(out=yb.h[:, c, :], in_=pxo.h[:, :], func=AF.Copy), [pxo.key], [YBK[c]])
                norm_residual(cx, lambda c: gain(l, 5, c), hh, 1.0)
                ffn(cx, l, 1, hh, 7, 8)
                store_h(cx, t)
            P.emit()
        lay.close()

    with ExitStack() as ph:
        xi = [sb("fi%d" % i, [128, DC, 128], F32, ph) for i in range(2)]
        xo2 = [sb("fo%d" % i, [128, D], F32, ph) for i in range(2)]
        for tb in range(N // 128):
            a = xi[tb % 2]
            o = xo2[tb % 2]
            P.dma("pool", a.h[:, :, :], hT[:, :, tb * 128:(tb + 1) * 128].rearrange("c p t -> p c t"), [], [a.key], a.g)
            for q in range(4):
                ps = psb[(tb * 4 + q) % 7]

                def tr(e, ps=ps, a=a, q=q):
                    for c in range(4):
                        ins = e.transpose(out=ps.h[:, c * 128:(c + 1) * 128], in_=a.h[:, q * 4 + c, :], identity=ident)
                    return ins

                P.op("pe", tr, [a.key, "cst"], [ps.key])
                if q % 2 == 0:
                    P.op("dve", lambda e, ps=ps, o=o, q=q: e.tensor_copy(out=o.h[:, q * 512:(q + 1) * 512], in_=ps.h[:, :]), [ps.key], [o.key])
                else:
                    P.op("act", lambda e, ps=ps, o=o, q=q: e.activation(out=o.h[:, q * 512:(q + 1) * 512], in_=ps.h[:, :], func=AF.Copy), [ps.key], [o.key])
            P.dma("pool", y_out[tb * 128:(tb + 1) * 128, :], o.h[:, :], [o.key], [], o.g)
        P.emit()
    P.stack.close()
    return nc


def host_consts():
    c = np.zeros((128, 704), np.float32)
    c[:, 0:128] = np.eye(128, dtype=np.float32)
    c[:, 128:256] = 1.0
    for m in range(128):
        if (m % 64) < 32:
            c[m + 32, 256 + m] = -1.0
        else:
            c[m - 32, 256 + m] = 1.0
    for a in range(HC):
        for b in range(HC):
            c[a, 384 + b] = 1.0 if a <= b else 0.0
            c[a, 416 + b] = 1.0 if a >= b else 0.0
    for i in range(32):
        c[i, 448 + 96 + i] = 1.0
    return c


def unit_inputs(x, mem2, N, single):
    n_seq = N if single else N // 2
    t = np.arange(N) % n_seq
    row = (t // 64).astype(np.float64)
    col = (t % 64).astype(np.float64)
    inv = 10000.0 ** (-np.arange(0, 64, 2, dtype=np.float64) / 64.0)
    ang = np.zeros((128, N))
    ang[0:32] = row[None, :] * inv[:, None]
    ang[32:64] = ang[0:32]
    ang[64:96] = col[None, :] * inv[:, None]
    ang[96:128] = ang[64:96]
    maskb = np.zeros((128, 4), np.float32)
    if not single:
        maskb[:, 1] = -30000.0
        maskb[:, 2] = -30000.0
    return {
        "x": np.ascontiguousarray(x, dtype=np.float32),
        "mem": np.ascontiguousarray(mem2, dtype=np.float32),
        "cos_t": np.cos(ang).astype(np.float32),
        "sin_t": np.sin(ang).astype(np.float32),
        "maskb": maskb,
        "rst": np.full((128, 1), 1.0 if single else 0.0, np.float32),
        "consts": host_consts(),
    }


_NC_CACHE = {}
UNIT_CORES = (0, 1, 4)
N_UNIT = 16384


def kernel(**inputs):
    import os
    dbg = tuple(x for x in os.environ.get("KDEBUG", "").split(",") if x)
    N = N_UNIT
    key = (N, dbg)
    if key not in _NC_CACHE:
        _NC_CACHE[key] = build(N, debug=dbg)
    nc = _NC_CACHE[key]
    xs, xp = inputs["x_sample"], inputs["x_prompt"]
    ms, mp = inputs["mem_sample"], inputs["mem_prompt"]
    wts = {k: np.ascontiguousarray(v, dtype=np.float32) for k, v in inputs.items()
           if k not in ("x_prompt", "x_sample", "mem_prompt", "mem_sample")}
    units = {
        UNIT_CORES[0]: unit_inputs(xs[0], np.stack([ms[0], ms[0]]), N, True),
        UNIT_CORES[1]: unit_inputs(xs[1], np.stack([ms[1], ms[1]]), N, True),
        UNIT_CORES[2]: unit_inputs(np.concatenate([xp[0], xp[1]], axis=0), np.stack([mp[0], mp[1]]), N, False),
    }
    idle = unit_inputs(np.zeros((N, D), np.float32), np.zeros((2, NMEM, D), np.float32), N, True)
    in_maps = []
    for c in range(8):
        m = dict(wts)
        m.update(units.get(c, idle))
        in_maps.append(m)
    res = run_bass_kernel_spmd(nc, in_maps, core_ids=list(range(8)))
    r = res.results
    y_sample = np.stack([r[UNIT_CORES[0]]["y"], r[UNIT_CORES[1]]["y"]]).astype(np.float32)
    yp = r[UNIT_CORES[2]]["y"]
    y_prompt = np.stack([yp[:N // 2], yp[N // 2:]]).astype(np.float32)
    return (y_prompt, y_sample)
```
